# Optimizing a Trainium2 kernel written in Bass

```python
import math
import jax, jax.numpy as jnp
from jax import lax
import numpy as np

D_MODEL = 1024
BATCH = 32
SEQ = 2048
DEPTH = 2

CTX_LEN = 256
GRID_W = 64
EPS = 1e-6

ATT_HEADS = 4
ATT_QK_DIM = 64
ATT_V_DIM = 2 * ATT_QK_DIM
ATT_WIDTH = ATT_HEADS * ATT_V_DIM
Q_BLOCK = 128
ROPE_THETA = 10000.0

REC_HEADS = 4
REC_DK = 128
REC_DV = 128
REC_KEY_WIDTH = REC_HEADS * REC_DK
REC_WIDTH = REC_HEADS * REC_DV
CHUNK = 64

D_FF = 2752
N_EXPERTS = 8
TOP_K = 2
D_FF_EXPERT = 3584
N_DENSE = (DEPTH + 1) // 2
N_MOE = DEPTH // 2

IN_SIZES = [ATT_HEADS * 2 * ATT_QK_DIM,
            ATT_HEADS * 2 * ATT_QK_DIM,
            ATT_WIDTH,
            REC_KEY_WIDTH,
            REC_WIDTH,
            REC_KEY_WIDTH,
            REC_KEY_WIDTH,
            REC_WIDTH,
            D_MODEL,
            D_MODEL]
D_IN = sum(IN_SIZES)
IN_SPLIT_POINTS = [int(v) for v in np.cumsum(IN_SIZES)[:-1]]

kernel_name = 'hybrid_diffattn_hgrn2_moe_dit'


def _rmsnorm(x, w):
    x32 = x.astype(jnp.float32)
    y = x32 * lax.rsqrt(jnp.mean(x32 * x32, axis=-1, keepdims=True) + EPS)
    return y.astype(x.dtype) * w


def _modulate(h, shift, scale):
    return h * (1 + scale) + shift


def _heads(a, dh):
    b, t, _ = a.shape
    return a.reshape(b, t, -1, dh).transpose(0, 2, 1, 3)


def _merge_heads(a):
    b, h, t, dh = a.shape
    return a.transpose(0, 2, 1, 3).reshape(b, t, h * dh)


def _rope_2d(rows):
    row = jnp.repeat(jnp.arange(rows, dtype=jnp.float32), GRID_W)
    col = jnp.tile(jnp.arange(GRID_W, dtype=jnp.float32), rows)
    n_freq = ATT_QK_DIM // 4
    inv_freq = ROPE_THETA ** (-jnp.arange(n_freq, dtype=jnp.float32) / n_freq)
    ang = jnp.concatenate([row[:, None] * inv_freq, col[:, None] * inv_freq], axis=-1)
    return jnp.cos(ang), jnp.sin(ang)


def _apply_rope(x, cos, sin):
    half = x.shape[-1] // 2
    cos = cos.astype(x.dtype)
    sin = sin.astype(x.dtype)
    xa, xb = x[..., :half], x[..., half:]
    return jnp.concatenate([xa * cos - xb * sin, xa * sin + xb * cos], axis=-1)


def _diff_attend(q, k, v, lam, scale):
    s = jnp.einsum('bhcqd,bhckd->bhcqk', q, k).astype(jnp.float32) * scale
    p = jax.nn.softmax(s, axis=-1)
    a = p[:, :, 0] - lam * p[:, :, 1]
    return jnp.einsum('bhqk,bhkd->bhqd', a.astype(v.dtype), v)


def _diff_attend_blocked(q, k, v, lam, scale):
    b, h, _, t, dq = q.shape
    nb = t // Q_BLOCK
    qb = jnp.moveaxis(q.reshape(b, h, 2, nb, Q_BLOCK, dq), 3, 0)
    out = lax.map(lambda qi: _diff_attend(qi, k, v, lam, scale), qb)
    return jnp.moveaxis(out, 0, 2).reshape(b, h, t, v.shape[-1])


def _log_forget(z, lb):
    z32 = z.astype(jnp.float32)
    lb32 = lb.astype(jnp.float32)
    return jnp.logaddexp(jnp.log(lb32), jnp.log1p(-lb32) + jax.nn.log_sigmoid(z32))


def _gla_scan(q, k, v, logf, s0):
    b, h, t, _ = q.shape
    dv = v.shape[-1]
    n = t // CHUNK

    def chunks(a):
        return jnp.moveaxis(a.astype(jnp.float32).reshape(b, h, n, CHUNK, a.shape[-1]), 2, 0)

    lower = jnp.tril(jnp.ones((CHUNK, CHUNK), dtype=bool))[:, :, None]

    def step(state, inp):
        qc, kc, vc, lfc = inp
        cum = jnp.cumsum(lfc, axis=2)
        rel = jnp.where(lower, cum[:, :, :, None, :] - cum[:, :, None, :, :], -jnp.inf)
        scores = jnp.einsum('bhtd,bhsd,bhtsd->bhts', qc, kc, jnp.exp(rel))
        o = (jnp.einsum('bhts,bhsv->bhtv', scores, vc)
             + jnp.einsum('bhtd,bhdv->bhtv', qc * jnp.exp(cum), state))
        last = cum[:, :, -1:, :]
        new_state = (jnp.exp(last[:, :, 0, :])[..., None] * state
                     + jnp.einsum('bhsd,bhsv->bhdv', kc * jnp.exp(last - cum), vc))
        return new_state, o

    s_fin, o = lax.scan(step, s0, (chunks(q), chunks(k), chunks(v), chunks(logf)))
    return jnp.moveaxis(o, 0, 2).reshape(b, h, t, dv), s_fin


def _layer_lower_bounds(lb_param):
    cs = jnp.cumsum(jax.nn.softmax(lb_param.astype(jnp.float32), axis=0), axis=0)
    return cs - cs[0:1]


def _mixer(h_lat, h_ctx, w_in, lq1, lk1, lq2, lk2, att_norm_w, rec_norm_w, lb_f, lb_b,
           w_up_att, w_up_rec, w_out, lam_init, cos, sin, need_ctx):
    dt = h_lat.dtype
    p_lat = jnp.split(h_lat @ w_in, IN_SPLIT_POINTS, axis=-1)
    p_ctx = jnp.split(h_ctx @ w_in, IN_SPLIT_POINTS, axis=-1)

    lam = (jnp.exp(jnp.sum(lq1.astype(jnp.float32) * lk1.astype(jnp.float32)))
           - jnp.exp(jnp.sum(lq2.astype(jnp.float32) * lk2.astype(jnp.float32))) + lam_init)
    scale = ATT_QK_DIM ** -0.5

    def att_qkv(parts):
        b, t, _ = parts[0].shape
        q = parts[0].reshape(b, t, ATT_HEADS, 2, ATT_QK_DIM).transpose(0, 2, 3, 1, 4)
        k = parts[1].reshape(b, t, ATT_HEADS, 2, ATT_QK_DIM).transpose(0, 2, 3, 1, 4)
        v = _heads(parts[2], ATT_V_DIM)
        return q, k, v

    q_c, k_c, v_c = att_qkv(p_ctx)
    q_l, k_l, v_l = att_qkv(p_lat)
    q_l = _apply_rope(q_l, cos, sin)
    k_l = _apply_rope(k_l, cos, sin)
    k_all = jnp.concatenate([k_c, k_l], axis=3)
    v_all = jnp.concatenate([v_c, v_l], axis=2)
    o_att_l = _diff_attend_blocked(q_l, k_all, v_all, lam, scale)

    def att_out(o):
        return _merge_heads(_rmsnorm(o, att_norm_w) * (1 - lam_init))

    def rec_inputs(parts):
        q = _heads(jax.nn.silu(parts[3]), REC_DK)
        v = _heads(parts[4], REC_DV)
        lf_f = _heads(_log_forget(parts[5], lb_f), REC_DK)
        lf_b = _heads(_log_forget(parts[6], lb_b), REC_DK)
        return q, v, lf_f, lf_b

    def flip(a):
        return jnp.flip(a, axis=2)

    qr_c, vr_c, lf_cf, lf_cb = rec_inputs(p_ctx)
    qr_l, vr_l, lf_lf, lf_lb = rec_inputs(p_lat)
    b = h_lat.shape[0]
    s0 = jnp.zeros((b, REC_HEADS, REC_DK, REC_DV), jnp.float32)
    o_cf, s_cf = _gla_scan(qr_c, -jnp.expm1(lf_cf), vr_c, lf_cf, s0)
    o_cb, s_cb = _gla_scan(flip(qr_c), flip(-jnp.expm1(lf_cb)), flip(vr_c), flip(lf_cb), s0)
    o_lf, _ = _gla_scan(qr_l, -jnp.expm1(lf_lf), vr_l, lf_lf, s_cf)
    o_lb, _ = _gla_scan(flip(qr_l), flip(-jnp.expm1(lf_lb)), flip(vr_l), flip(lf_lb), s_cb)
    o_rec_l = o_lf + flip(o_lb)

    def rec_out(o, g):
        return _merge_heads(_rmsnorm(o.astype(dt), rec_norm_w)) * jax.nn.silu(g)

    def merge(y_att, y_rec, parts):
        g_att = jax.nn.sigmoid(parts[8])
        g_rec = jax.nn.sigmoid(parts[9])
        return (g_att * (y_att @ w_up_att) + g_rec * (y_rec @ w_up_rec)) @ w_out

    y_lat = merge(att_out(o_att_l), rec_out(o_rec_l, p_lat[7]), p_lat)
    y_ctx = None
    if need_ctx:
        o_att_c = _diff_attend(q_c, k_c, v_c, lam, scale)
        y_ctx = merge(att_out(o_att_c), rec_out(o_cf + flip(o_cb), p_ctx[7]), p_ctx)
    return y_lat, y_ctx


def _swiglu(h, w1, w3, w2):
    return (jax.nn.silu(h @ w1) * (h @ w3)) @ w2


def _moe(h, w_router, w1, w3, w2):
    logits = (h @ w_router).astype(jnp.float32)
    vals, idx = lax.top_k(logits, TOP_K)
    wts = jax.nn.softmax(vals, axis=-1)
    gates = jnp.sum(jax.nn.one_hot(idx, N_EXPERTS, dtype=jnp.float32) * wts[..., None], axis=-2).astype(h.dtype)
    out = jnp.zeros_like(h)
    for e in range(N_EXPERTS):
        out = out + gates[..., e:e + 1] * _swiglu(h, w1[e], w3[e], w2[e])
    return out


def setup_inputs(seed: int = 0) -> dict:
    key = jax.random.key(seed)
    ks = iter(jax.random.split(key, 40))

    def nrm(shape, scale):
        return jax.random.normal(next(ks), shape, jnp.float32) * scale

    def gain(shape):
        return 1.0 + nrm(shape, 0.05)

    D = D_MODEL
    return {
        'x': nrm((BATCH, SEQ, D), 1.0),
        'c': nrm((BATCH, D), 1.0),
        'ctx': nrm((BATCH, CTX_LEN, D), 1.0),
        'c_ctx': nrm((D,), 1.0),
        'w_ada': nrm((DEPTH, D, 6 * D), 0.5 * D ** -0.5),
        'b_ada': nrm((DEPTH, 6 * D), 0.02),
        'norm_mix_w': gain((DEPTH, D)),
        'norm_ffn_w': gain((DEPTH, D)),
        'w_in': nrm((DEPTH, D, D_IN), D ** -0.5),
        'lambda_q1': nrm((DEPTH, ATT_QK_DIM), 0.1),
        'lambda_k1': nrm((DEPTH, ATT_QK_DIM), 0.1),
        'lambda_q2': nrm((DEPTH, ATT_QK_DIM), 0.1),
        'lambda_k2': nrm((DEPTH, ATT_QK_DIM), 0.1),
        'att_norm_w': gain((DEPTH, ATT_V_DIM)),
        'rec_norm_w': gain((DEPTH, REC_DV)),
        'lb_fwd': 1.0 + nrm((DEPTH, REC_KEY_WIDTH), 0.5),
        'lb_bwd': 1.0 + nrm((DEPTH, REC_KEY_WIDTH), 0.5),
        'w_up_att': nrm((DEPTH, ATT_WIDTH, D), ATT_WIDTH ** -0.5),
        'w_up_rec': nrm((DEPTH, REC_WIDTH, D), REC_WIDTH ** -0.5),
        'w_out': nrm((DEPTH, D, D), D ** -0.5),
        'ffn_w1': nrm((N_DENSE, D, D_FF), D ** -0.5),
        'ffn_w3': nrm((N_DENSE, D, D_FF), D ** -0.5),
        'ffn_w2': nrm((N_DENSE, D_FF, D), D_FF ** -0.5),
        'router_w': nrm((N_MOE, D, N_EXPERTS), D ** -0.5),
        'moe_w1': nrm((N_MOE, N_EXPERTS, D, D_FF_EXPERT), D ** -0.5),
        'moe_w3': nrm((N_MOE, N_EXPERTS, D, D_FF_EXPERT), D ** -0.5),
        'moe_w2': nrm((N_MOE, N_EXPERTS, D_FF_EXPERT, D), D_FF_EXPERT ** -0.5),
        'final_norm_w': gain((D,)),
    }


def reference(x, c, ctx, c_ctx, w_ada, b_ada, norm_mix_w, norm_ffn_w, w_in,
              lambda_q1, lambda_k1, lambda_q2, lambda_k2, att_norm_w, rec_norm_w,
              lb_fwd, lb_bwd, w_up_att, w_up_rec, w_out, ffn_w1, ffn_w3, ffn_w2,
              router_w, moe_w1, moe_w3, moe_w2, final_norm_w):
    rows = x.shape[1] // GRID_W
    cos, sin = _rope_2d(rows)
    lbs_f = _layer_lower_bounds(lb_fwd)
    lbs_b = _layer_lower_bounds(lb_bwd)
    silu_c = jax.nn.silu(c)
    silu_cc = jax.nn.silu(c_ctx)

    for l in range(DEPTH):
        last = l == DEPTH - 1
        mod_l = [m[:, None, :] for m in jnp.split(silu_c @ w_ada[l] + b_ada[l], 6, axis=-1)]
        mod_c = jnp.split(silu_cc @ w_ada[l] + b_ada[l], 6, axis=-1)
        lam_init = 0.8 - 0.6 * math.exp(-0.3 * l)

        h_l = _modulate(_rmsnorm(x, norm_mix_w[l]), mod_l[0], mod_l[1])
        h_c = _modulate(_rmsnorm(ctx, norm_mix_w[l]), mod_c[0], mod_c[1])
        y_l, y_c = _mixer(h_l, h_c, w_in[l], lambda_q1[l], lambda_k1[l], lambda_q2[l], lambda_k2[l],
                          att_norm_w[l], rec_norm_w[l], lbs_f[l], lbs_b[l],
                          w_up_att[l], w_up_rec[l], w_out[l], lam_init, cos, sin, not last)
        x = x + mod_l[2] * y_l
        if not last:
            ctx = ctx + mod_c[2] * y_c

        if l % 2 == 0:
            j = l // 2
            ffn = lambda h, j=j: _swiglu(h, ffn_w1[j], ffn_w3[j], ffn_w2[j])
        else:
            j = l // 2
            ffn = lambda h, j=j: _moe(h, router_w[j], moe_w1[j], moe_w3[j], moe_w2[j])
        x = x + mod_l[5] * ffn(_modulate(_rmsnorm(x, norm_ffn_w[l]), mod_l[3], mod_l[4]))
        if not last:
            ctx = ctx + mod_c[5] * ffn(_modulate(_rmsnorm(ctx, norm_ffn_w[l]), mod_c[3], mod_c[4]))

    return _rmsnorm(x, final_norm_w)
```

```python
import math
import numpy as np
import ml_dtypes
import concourse.bass as bass
import concourse.mybir as mybir
from concourse.bass_utils import run_bass_kernel_spmd

F32 = mybir.dt.float32
BF16 = mybir.dt.bfloat16
AF = mybir.ActivationFunctionType
ALU = mybir.AluOpType
AX = mybir.AxisListType


class Buf:
    __slots__ = ("name", "w", "readers")

    def __init__(self, name=""):
        self.name = name
        self.w = None
        self.readers = []


class Eng:
    def __init__(self, fw, name, handle, compute=True):
        self.fw = fw
        self.name = name
        self.h = handle
        self.compute = compute
        self.sem = None
        self.count = 0
        self.known = {}
        self.nsem = 0
        self.pending = False
        if compute:
            self._new_sem()

    def _new_sem(self):
        self.sem = self.fw.new_sem(f"{self.name}{self.nsem}")
        self.nsem += 1
        self.count = 0

    def wait(self, tok):
        sem, val, eng = tok
        k = id(sem)
        if self.known.get(k, 0) >= val:
            return
        if eng is self and eng.compute and sem is eng.sem and val > eng.count:
            raise RuntimeError(f"{self.name}: waiting on own future token")
        self.h.wait_ge(sem, val)
        self.known[k] = val


class FW:
    ROLL = 30000

    def __init__(self, nc):
        self.nc = nc
        self._stack = []
        self._sems = []
        self.pe = Eng(self, "pe", nc.tensor)
        self.act = Eng(self, "act", nc.scalar)
        self.dve = Eng(self, "dve", nc.vector)
        self.pool = Eng(self, "pool", nc.gpsimd)
        self.sp = Eng(self, "sp", nc.sync)
        self.engs = [self.pe, self.act, self.dve, self.pool, self.sp]
        self.dma_rings = {}
        self.n_inst = 0

    def new_sem(self, name):
        cm = self.nc.semaphore(name)
        s = cm.__enter__()
        self._stack.append(cm)
        self._sems.append(s)
        return s

    def close(self):
        for cm in reversed(self._stack):
            cm.__exit__(None, None, None)
        self._stack = []

    def op(self, eng, fn, reads=(), writes=(), inc=True):
        toks = {}
        for b in reads:
            if b.w is not None:
                toks[(id(b.w[0]), b.w[1])] = b.w
        for b in writes:
            if b.w is not None:
                toks[(id(b.w[0]), b.w[1])] = b.w
            for r in b.readers:
                toks[(id(r[0]), r[1])] = r
        rtoks = set()
        for b_ in reads:
            if b_.w is not None:
                rtoks.add((id(b_.w[0]), b_.w[1]))
        for k_, t in toks.items():
            if t[2] is eng and eng is self.pe:
                continue
            if t[2] is eng and k_ not in rtoks:
                continue
            if t[2] is eng and t[0] is eng.sem and t[1] > eng.count:
                continue
            eng.wait(t)
        inst = fn()
        self.n_inst += 1
        if inc:
            if eng.count >= self.ROLL and not eng.pending:
                eng._new_sem()
            eng.count += 1
            eng.pending = False
            inst.then_inc(eng.sem, 1)
            tok = (eng.sem, eng.count, eng)
        else:
            eng.pending = True
            tok = (eng.sem, eng.count + 1, eng)
        for b in reads:
            b.readers = [r for r in b.readers if r[2] is not eng or r[0] is not tok[0]]
            b.readers.append(tok)
        for b in writes:
            b.w = tok
            b.readers = []
        return inst

    def dma(self, q, out, in_, reads=(), writes=(), **kw):
        ring = self.dma_rings.get(q.name)
        if ring is None:
            ring = {"sems": [self.new_sem(f"d{q.name}{i}") for i in range(12)],
                    "cnt": [0] * 12, "i": 0}
            self.dma_rings[q.name] = ring
        i = ring["i"]
        ring["i"] = (i + 1) % len(ring["sems"])
        sem = ring["sems"][i]
        toks = {}
        for b in reads:
            if b.w is not None:
                toks[(id(b.w[0]), b.w[1])] = b.w
        for b in writes:
            if b.w is not None:
                toks[(id(b.w[0]), b.w[1])] = b.w
            for r in b.readers:
                toks[(id(r[0]), r[1])] = r
        if ring["cnt"][i] > 0:
            toks[(id(sem), ring["cnt"][i])] = (sem, ring["cnt"][i], None)
        for t in toks.values():
            q.wait(t)
        if ring["cnt"][i] + 16 > self.ROLL:
            sem = self.new_sem(f"d{q.name}{i}r{self.n_inst}")
            ring["sems"][i] = sem
            ring["cnt"][i] = 0
        inst = q.h.dma_start(out=out, in_=in_, **kw)
        self.n_inst += 1
        ring["cnt"][i] += 16
        inst.then_inc(sem, 16)
        tok = (sem, ring["cnt"][i], None)
        for b in reads:
            b.readers.append(tok)
        for b in writes:
            b.w = tok
            b.readers = []
        return tok

    def barrier(self):
        toks = []
        for e in self.engs:
            if e.count > 0:
                toks.append((e.sem, e.count, e))
        for ring in self.dma_rings.values():
            for sem, cnt in zip(ring["sems"], ring["cnt"]):
                if cnt > 0:
                    toks.append((sem, cnt, None))
        for e in self.engs:
            for t in toks:
                if t[2] is e:
                    continue
                e.wait(t)

    def finish(self, toks, eng=None):
        eng = eng or self.sp
        for t in toks:
            eng.wait(t)


class Ring:
    def __init__(self, alloc, name, shape, dtype, n):
        self.items = [(alloc(f"{name}{i}", shape, dtype), Buf(f"{name}{i}")) for i in range(n)]
        self.i = 0

    def next(self):
        it = self.items[self.i]
        self.i = (self.i + 1) % len(self.items)
        return it


D = 1024
KC = 8
DIN = 6144
NE = 8
EPS = 1e-6
CH = 32
GRID_W = 64


def host_consts(T):
    c = {}
    ident = np.eye(128, dtype=np.float32)
    s = np.arange(128)
    same = (s[:, None] // CH) == (s[None, :] // CH)
    Lf = (same & (s[:, None] <= s[None, :])).astype(np.float32)
    Lb = (same & (s[:, None] >= s[None, :])).astype(np.float32)
    ind = (s[:, None] // CH == np.arange(4)[None, :]).astype(np.float32)
    RT = np.zeros((128, 128), np.float32)
    for m in range(128):
        if m % 64 < 32:
            RT[m + 32, m] = -1.0
        else:
            RT[m - 32, m] = 1.0
    c["cmat"] = np.concatenate([ident, Lf, Lb, RT, ind, np.ones((128, 4), np.float32)], axis=1)
    rows = T // GRID_W
    row = np.repeat(np.arange(rows, dtype=np.float32), GRID_W)
    col = np.tile(np.arange(GRID_W, dtype=np.float32), rows)
    nf = 16
    inv = (10000.0 ** (-np.arange(nf, dtype=np.float32) / nf)).astype(np.float32)
    ang = np.concatenate([row[:, None] * inv, col[:, None] * inv], axis=-1).astype(np.float32)
    cosT = np.cos(ang).astype(np.float32).T
    sinT = np.sin(ang).astype(np.float32).T
    c["cos"] = np.ascontiguousarray(np.tile(cosT, (4, 1)))
    c["sin"] = np.ascontiguousarray(np.tile(sinT, (4, 1)))
    return c


def build_program(NB, T, C, DFF, DFE, depth=2, stop_after=None, dbg=False):
    nc = bass.Bass("TRN2", target_bir_lowering=False)
    fw = FW(nc)
    pe, act, dve, pool, sp = fw.pe, fw.act, fw.dve, fw.pool, fw.sp
    TT = C + T
    NT = TT // 128
    NCT = C // 128
    NLT = T // 128
    L = depth
    n_dense = (L + 1) // 2
    n_moe = L // 2

    def din(name, shape, dt=F32):
        return nc.dram_tensor(name, list(shape), dt, kind="ExternalInput").ap()

    def dscr(name, shape, dt):
        if dbg and name in ("mods_s", "XS", "QT", "KT", "VS", "REC", "GT", "YT", "OB"):
            return nc.dram_tensor(name, list(shape), dt, kind="ExternalOutput").ap()
        return nc.dram_tensor(name, list(shape), dt).ap()

    x_d = din("x", [NB, T, D]); ctx_d = din("ctx", [NB, C, D]); cc_d = din("cc", [NB + 1, D])
    w_ada = din("w_ada", [L, D, 6 * D]); b_ada = din("b_ada", [L, 6 * D])
    nmw = din("norm_mix_w", [L, D]); nfw = din("norm_ffn_w", [L, D])
    w_in = din("w_in", [L, D, DIN])
    lq1 = din("lambda_q1", [L, 64]); lk1 = din("lambda_k1", [L, 64]); lq2 = din("lambda_q2", [L, 64]); lk2 = din("lambda_k2", [L, 64])
    anw = din("att_norm_w", [L, 128]); rnw = din("rec_norm_w", [L, 128])
    lbf = din("lb_fwd", [L, 512]); lbb = din("lb_bwd", [L, 512])
    wua = din("w_up_att", [L, 512, D]); wur = din("w_up_rec", [L, 512, D]); wout = din("w_out", [L, D, D])
    fw1 = din("ffn_w1", [n_dense, D, DFF]); fw3 = din("ffn_w3", [n_dense, D, DFF]); fw2 = din("ffn_w2", [n_dense, DFF, D])
    rtw = din("router_w", [n_moe, D, NE])
    mw1 = din("moe_w1", [n_moe, NE, D, DFE]); mw3 = din("moe_w3", [n_moe, NE, D, DFE]); mw2 = din("moe_w2", [n_moe, NE, DFE, D])
    fnw = din("final_norm_w", [1, D])
    cmat_d = din("cmat", [128, 520]); cos_d = din("cos", [128, T]); sin_d = din("sin", [128, T])
    out_d = nc.dram_tensor("out", [NB, T, D], F32, kind="ExternalOutput").ap()
    dbg_d = nc.dram_tensor("dbg", [TT, D], F32, kind="ExternalOutput").ap() if dbg else None

    w_in_b = dscr("w_in_b", [L, D, DIN], BF16)
    wua_b = dscr("wua_b", [L, 512, D], BF16); wur_b = dscr("wur_b", [L, 512, D], BF16); wout_b = dscr("wout_b", [L, D, D], BF16)
    fw1_b = dscr("fw1_b", [n_dense, D, DFF], BF16); fw3_b = dscr("fw3_b", [n_dense, D, DFF], BF16); fw2_b = dscr("fw2_b", [n_dense, DFF, D], BF16)
    mw1_b = dscr("mw1_b", [n_moe, NE, D, DFE], BF16); mw3_b = dscr("mw3_b", [n_moe, NE, D, DFE], BF16); mw2_b = dscr("mw2_b", [n_moe, NE, DFE, D], BF16)
    rtw_b = dscr("rtw_b", [n_moe, D, NE], BF16)
    mods_s = dscr("mods_s", [L, NB + 1, 6 * D], F32)
    XS = dscr("XS", [TT, D], F32)
    QT = dscr("QT", [4, 128, TT], BF16); KT = dscr("KT", [4, 128, TT], BF16)
    VS = dscr("VS", [TT, 512], BF16)
    REC = dscr("REC", [TT, 5, 512], F32)
    GT = dscr("GT", [16, 128, TT], BF16)
    YT = dscr("YT", [8, 128, TT], BF16)
    OB = dscr("OB", [TT, 512], F32)

    ARENA = 52000
    arena = nc.alloc_sbuf_tensor("arena", [128, ARENA], F32).ap()
    reg = {"cur": 0, "hi": 0, "top": ARENA, "persist": True}

    def sbt(name, shape, dt):
        shape = list(shape)
        n = int(np.prod(shape[1:]))
        w = (n + 1) // 2 if dt == BF16 else n
        w = (w + 7) // 8 * 8
        if reg["persist"]:
            reg["top"] -= w
            o = reg["top"]
        else:
            o = reg["cur"]
            reg["cur"] = o + w
            reg["hi"] = max(reg["hi"], reg["cur"])
        assert reg["hi"] <= reg["top"], f"SBUF arena overflow at {name}: {reg['hi']} > {reg['top']}"
        v = arena[0:shape[0], o:o + ((n + 1) // 2 if dt == BF16 else n)]
        if dt == BF16:
            v = v.bitcast(BF16)[:, 0:n]
        if len(shape) == 3:
            v = v.rearrange("p (a b) -> p a b", a=shape[1])
        elif len(shape) == 4:
            v = v.rearrange("p (a b c) -> p a b c", a=shape[1], b=shape[2])
        return v

    def phase_region():
        reg["cur"] = 0
        reg["persist"] = False

    def persist_region():
        reg["persist"] = True
    pst = lambda n, s, d: nc.alloc_psum_tensor(n, list(s), d).ap()

    PS = [(pst(f"ps{i}", [128, 512], F32), Buf(f"ps{i}")) for i in range(8)]
    ps_rr = [0]

    def psum(lo=0, hi=8):
        i = ps_rr[0] % (hi - lo) + lo
        ps_rr[0] += 1
        return PS[i]

    out_toks = []

    cmat = sbt("cmat", [128, 520], F32); b_cm = Buf()
    fw.dma(sp, cmat, cmat_d, writes=[b_cm])
    identf = cmat[:, 0:128]; Lf_f = cmat[:, 128:256]; Lb_f = cmat[:, 256:384]; RTf = cmat[:, 384:512]; indf = cmat[:, 512:516]
    cbf = sbt("cbf", [128, 520], BF16); b_cb = Buf()
    fw.op(dve, lambda: nc.vector.tensor_copy(cbf, cmat), reads=[b_cm], writes=[b_cb])
    identb = cbf[:, 0:128]
    nmask = sbt("nmask", [128, 2, 128], F32); b_nm = Buf()
    fw.op(dve, lambda: nc.vector.tensor_scalar(nmask.rearrange("p a b -> p (a b)"), cmat[:, 128:384], -1.0, None, ALU.mult),
          reads=[b_cm], writes=[b_nm])
    epsT = sbt("epsT", [128, 1], F32); b_eps = Buf()
    fw.op(pool, lambda: nc.gpsimd.memset(epsT, EPS), writes=[b_eps])

    wb = Buf("weights_b")

    def flat2(ap, cols):
        nd = len(ap.shape)
        names = " ".join(f"d{i}" for i in range(nd))
        f = ap.rearrange(f"{names} -> ({names})")
        return f.rearrange("(r c) -> r c", c=cols)

    conv_list = [(w_in_b, w_in), (wua_b, wua), (wur_b, wur), (wout_b, wout), (fw1_b, fw1), (fw3_b, fw3), (fw2_b, fw2),
                 (mw1_b, mw1), (mw3_b, mw3), (mw2_b, mw2), (rtw_b, rtw)]
    conv_toks = []
    for dst, src in conv_list:
        n = int(np.prod(dst.shape))
        if n == 0:
            continue
        cols = 2048
        while n % cols:
            cols //= 2
        d2 = flat2(dst, cols); s2 = flat2(src, cols)
        rows = n // cols
        step = 2048
        for r0 in range(0, rows, step):
            r1 = min(rows, r0 + step)
            b = Buf()
            conv_toks.append(fw.dma(pool, d2[r0:r1, :], s2[r0:r1, :], writes=[b]))
    for t in conv_toks:
        sp.wait(t)

    NR = NB + 1
    ones_row = sbt("ones_row", [1, 8], F32); b_or = Buf()
    phase_region()
    ccs = sbt("ccs", [NR, D], F32); b_cc = Buf()
    fw.dma(sp, ccs, cc_d, writes=[b_cc])
    fw.op(act, lambda: nc.scalar.activation(ccs, ccs, AF.Silu), reads=[b_cc], writes=[b_cc])
    scT = sbt("scT", [128, KC, NR], F32); b_scT = Buf()
    pt, bpt = psum()
    for kc in range(KC):
        fw.op(pe, lambda kc=kc: nc.tensor.transpose(pt[:, kc * NR:(kc + 1) * NR], ccs[:, kc * 128:(kc + 1) * 128], identf[0:NR, 0:NR]),
              reads=[b_cc, b_cm], writes=[bpt], inc=(kc == KC - 1))
    fw.op(act, lambda: nc.scalar.copy(scT.rearrange("p a b -> p (a b)"), pt[:, 0:KC * NR]), reads=[bpt], writes=[b_scT])
    onesr = cmat[0:1, 516:516 + 1]
    fw.op(pool, lambda: nc.gpsimd.memset(ones_row, 1.0), writes=[b_or])
    adar = Ring(sbt, "adaw", [128, KC, 512], F32, 2)
    bar = Ring(sbt, "adab", [1, 512], F32, 2)
    mor = Ring(sbt, "adao", [NR, 512], F32, 2)
    b_mods = Buf("mods")
    mods_toks = []
    for l in range(L):
        for j in range(12):
            wsl, bw = adar.next()
            fw.dma(sp, wsl, w_ada[l].rearrange("(kc p) n -> p kc n", p=128)[:, :, j * 512:(j + 1) * 512], writes=[bw])
            bsl, bb = bar.next()
            fw.dma(sp, bsl, b_ada[l:l + 1, j * 512:(j + 1) * 512], writes=[bb])
            pm, bpm = psum()
            for kc in range(KC):
                fw.op(pe, lambda kc=kc: nc.tensor.matmul(pm[0:NR, :], scT[:, kc, :], wsl[:, kc, :], start=(kc == 0), stop=False),
                      reads=[b_scT, bw], writes=[bpm], inc=False)
            fw.op(pe, lambda: nc.tensor.matmul(pm[0:NR, :], ones_row[0:1, 0:NR], bsl, start=False, stop=True),
                  reads=[b_or, bb], writes=[bpm])
            mo, bmo = mor.next()
            fw.op(act, lambda: nc.scalar.copy(mo, pm[0:NR, :]), reads=[bpm], writes=[bmo])
            mods_toks.append(fw.dma(sp, mods_s[l, :, j * 512:(j + 1) * 512], mo, reads=[bmo], writes=[Buf()]))
    for t in mods_toks:
        sp.wait(t)

    bc = lambda ap_row: ap_row.partition_broadcast(128)

    persist_region()
    lamT = sbt("lamT", [128, 8], F32); b_lam = Buf()
    lqk = sbt("lqk", [128, 4, 64], F32); b_lqk = Buf()
    ANW = sbt("ANW", [128, 128], F32); RNW = sbt("RNW", [128, 128], F32); b_nw = Buf()
    b_lb = Buf(); b_lbt = Buf(); b_fnw = Buf()

    def layer_consts(l):
        lam_init = 0.8 - 0.6 * math.exp(-0.3 * l)
        for i, a in enumerate([lq1, lk1, lq2, lk2]):
            fw.dma(sp, lqk[:, i, :], bc(a[l:l + 1, :]), writes=[b_lqk])
        fw.op(dve, lambda: nc.vector.tensor_tensor(lqk[:, 0, :], lqk[:, 0, :], lqk[:, 1, :], ALU.mult), reads=[b_lqk], writes=[b_lqk])
        fw.op(dve, lambda: nc.vector.tensor_tensor(lqk[:, 2, :], lqk[:, 2, :], lqk[:, 3, :], ALU.mult), reads=[b_lqk], writes=[b_lqk])
        fw.op(dve, lambda: nc.vector.reduce_sum(lamT[:, 0:1], lqk[:, 0, :], AX.X), reads=[b_lqk, b_lam], writes=[b_lam])
        fw.op(dve, lambda: nc.vector.reduce_sum(lamT[:, 1:2], lqk[:, 2, :], AX.X), reads=[b_lqk, b_lam], writes=[b_lam])
        fw.op(act, lambda: nc.scalar.activation(lamT[:, 2:4], lamT[:, 0:2], AF.Exp), reads=[b_lam], writes=[b_lam])
        fw.op(dve, lambda: nc.vector.tensor_tensor(lamT[:, 4:5], lamT[:, 2:3], lamT[:, 3:4], ALU.subtract), reads=[b_lam], writes=[b_lam])
        fw.op(dve, lambda: nc.vector.tensor_scalar(lamT[:, 5:6], lamT[:, 4:5], lam_init, -1.0, ALU.add, ALU.mult), reads=[b_lam], writes=[b_lam])
        fw.dma(sp, ANW, bc(anw[l:l + 1, :]), writes=[b_nw])
        fw.dma(sp, RNW, bc(rnw[l:l + 1, :]), writes=[b_nw])
        fw.op(dve, lambda: nc.vector.tensor_scalar(ANW, ANW, 1.0 - lam_init, None, ALU.mult), reads=[b_nw], writes=[b_nw])
    def lb_consts(l):
        for d_, a in enumerate([lbf, lbb]):
            if l == 0:
                fw.op(dve, lambda d_=d_: nc.vector.memset(LBt[:, d_, 0, :], 0.0), writes=[b_lb])
                fw.op(dve, lambda d_=d_: nc.vector.memset(LBt[:, d_, 1, :], 1.0), writes=[b_lb])
            else:
                assert L == 2 and l == 1
                fw.dma(sp, lbtmp[:, 0, :], bc(a[0:1, :]), writes=[b_lbt])
                fw.dma(sp, lbtmp[:, 1, :], bc(a[1:2, :]), writes=[b_lbt])
                fw.op(dve, lambda: nc.vector.tensor_tensor(lbtmp[:, 0, :], lbtmp[:, 1, :], lbtmp[:, 0, :], ALU.subtract), reads=[b_lbt], writes=[b_lbt])
                fw.op(act, lambda d_=d_: nc.scalar.activation(LBt[:, d_, 0, :], lbtmp[:, 0, :], AF.Sigmoid), reads=[b_lbt, b_lb], writes=[b_lb])
                fw.op(dve, lambda d_=d_: nc.vector.tensor_scalar(LBt[:, d_, 1, :], LBt[:, d_, 0, :], -1.0, 1.0, ALU.mult, ALU.add), reads=[b_lb], writes=[b_lb])

    def load_mod(t, bt, l, row, j):
        fw.dma(sp, t, bc(mods_s[l, row:row + 1, j * D:(j + 1) * D]), writes=[bt])

    def load_gain_shift(G, bG, S, bS, l, row, jscale, jshift, nw):
        fw.dma(sp, S, bc(nw[l:l + 1, :]), writes=[bS])
        load_mod(G, bG, l, row, jscale)
        fw.op(dve, lambda: nc.vector.scalar_tensor_tensor(G, G, 1.0, S, ALU.add, ALU.mult), reads=[bG, bS], writes=[bG])
        load_mod(S, bS, l, row, jshift)

    ssr = Ring(sbt, "ss", [128, 4], F32, 4)
    junk = Ring(sbt, "junk", [128, D], BF16, 2)
    tmpr = Ring(sbt, "nrm_t", [128, D], F32, 2)

    def rstd_of(xt, bx, n, width):
        ss, bss = ssr.next()
        jk, bjk = junk.next()
        fw.op(dve, lambda: nc.vector.memset(ss, 0.0), writes=[bss])
        fw.op(act, lambda: nc.scalar.activation(jk[:, 0:width], xt, AF.Square, accum_out=ss[:, 0:1]), reads=[bx, bss], writes=[bjk, bss])
        fw.op(act, lambda: nc.scalar.activation(ss[:, 1:2], ss[:, 0:1], AF.Sqrt, bias=epsT[:, 0:1], scale=1.0 / n), reads=[bss, b_eps], writes=[bss])
        fw.op(dve, lambda: nc.vector.reciprocal(ss[:, 2:3], ss[:, 1:2]), reads=[bss], writes=[bss])
        return ss[:, 2:3], bss

    def norm_mod(xt, bx, G, bG, S, bS, hb, bhb):
        r, br = rstd_of(xt, bx, D, D)
        t, bt = tmpr.next()
        fw.op(dve, lambda: nc.vector.scalar_tensor_tensor(t, xt, r, G, ALU.mult, ALU.mult), reads=[bx, br, bG], writes=[bt])
        fw.op(pool, lambda: nc.gpsimd.tensor_tensor(hb, t, S, ALU.add), reads=[bt, bS], writes=[bhb])

    xr = Ring(sbt, "xt", [128, D], F32, 2)
    hbr = Ring(sbt, "hb", [128, D], BF16, 2)
    xor_ = Ring(sbt, "xo", [128, D], F32, 2)
    f32r = Ring(sbt, "f32t", [128, 512], F32, 4)
    bf16r = Ring(sbt, "bf16t", [128, 512], BF16, 4)
    phase_region()
    hT = sbt("hT", [128, KC, TT], BF16); b_hT = [Buf(f"hT{i}") for i in range(NT)]
    cosT = sbt("cosT", [128, T], F32); sinT = sbt("sinT", [128, T], F32); b_cs = Buf()
    slabr = Ring(sbt, "wslab", [128, KC, 512], BF16, 2)
    M1 = {k: (sbt(f"m1_{k}", [128, D], F32), Buf(k)) for k in ["G1l", "S1l", "G1c", "S1c"]}
    XSb = [Buf(f"XS{i}") for i in range(NT)]

    def xsrc(l, b, i):
        if l == 0:
            if i < NCT:
                return ctx_d[b, i * 128:(i + 1) * 128, :]
            return x_d[b, (i - NCT) * 128:(i - NCT + 1) * 128, :]
        return XS[i * 128:(i + 1) * 128, :]

    def transpose_to(dst3, bdst, src, bsrc, nchunk, lo=0, hi=8):
        ptt, bptt = psum(lo, hi)
        pv = ptt.bitcast(BF16)
        for kc in range(nchunk):
            fw.op(pe, lambda kc=kc: nc.tensor.transpose(pv[:, kc * 128:(kc + 1) * 128], src[:, kc * 128:(kc + 1) * 128], identb),
                  reads=[bsrc, b_cb], writes=[bptt], inc=(kc == nchunk - 1))
        fw.op(act, lambda: nc.scalar.copy(dst3, pv[:, 0:nchunk * 128].rearrange("p (k t) -> p k t", k=nchunk)),
              reads=[bptt], writes=[bdst])

    blocks_all = [(0, C)] + [(C + i * 512, min(512, TT - C - i * 512)) for i in range((T + 511) // 512)]
    QTb = [[Buf() for _ in blocks_all] for _ in range(4)]
    KTb = [[Buf() for _ in blocks_all] for _ in range(4)]
    VSb = [Buf() for _ in range(NT)]
    RECb = [[Buf() for _ in range(5)] for _ in range(NT)]
    GTb = [[Buf() for _ in blocks_all] for _ in range(16)]
    YTb = [[Buf() for _ in range(NT)] for _ in range(8)]
    OBb = [Buf() for _ in range(NT)]

    def phase1(l, b):
        load_gain_shift(*M1["G1l"], *M1["S1l"], l, b, 1, 0, nmw)
        load_gain_shift(*M1["G1c"], *M1["S1c"], l, NB, 1, 0, nmw)
        for i in range(NT):
            xt, bx = xr.next()
            src_b = XSb[i] if l > 0 else Buf()
            fw.dma(sp, xt, xsrc(l, b, i), reads=[src_b], writes=[bx])
            suf = "c" if i < NCT else "l"
            hb, bhb = hbr.next()
            norm_mod(xt, bx, *M1["G1" + suf], *M1["S1" + suf], hb, bhb)
            transpose_to(hT[:, :, i * 128:(i + 1) * 128], b_hT[i], hb, bhb, KC)

    def phase1b(l, b):
        wv = w_in_b[l].rearrange("(kc p) n -> p kc n", p=128)
        fw.dma(sp, cosT, cos_d, writes=[b_cs]); fw.dma(sp, sinT, sin_d, writes=[b_cs])
        for s in range(12):
            sl, bsl = slabr.next()
            fw.dma(sp, sl, wv[:, :, s * 512:(s + 1) * 512], writes=[bsl])
            if s in (0, 1, 8, 9, 10, 11):
                for cc in range(4):
                    for bi, (t0, n) in enumerate(blocks_all):
                        tiles = range(t0 // 128, (t0 + n) // 128)
                        pp, bpp = psum()
                        for kc in range(KC):
                            fw.op(pe, lambda kc=kc: nc.tensor.matmul(pp[:, 0:n], sl[:, kc, cc * 128:(cc + 1) * 128], hT[:, kc, t0:t0 + n],
                                                                     start=(kc == 0), stop=(kc == KC - 1)),
                                  reads=[bsl] + [b_hT[i] for i in tiles], writes=[bpp], inc=(kc == KC - 1))
                        ob, bob = bf16r.next()
                        if s >= 8:
                            g = (s - 8) * 4 + cc
                            fw.op(act, lambda: nc.scalar.activation(ob[:, 0:n], pp[:, 0:n], AF.Sigmoid), reads=[bpp], writes=[bob])
                            fw.dma(pool, GT[g, :, t0:t0 + n], ob[:, 0:n], reads=[bob], writes=[GTb[g][bi]])
                        else:
                            dst, dstb = (QT, QTb) if s == 0 else (KT, KTb)
                            if bi == 0:
                                fw.op(act, lambda: nc.scalar.copy(ob[:, 0:n], pp[:, 0:n]), reads=[bpp], writes=[bob])
                            else:
                                xq, bxq = f32r.next()
                                fw.op(act, lambda: nc.scalar.copy(xq[:, 0:n], pp[:, 0:n]), reads=[bpp], writes=[bxq])
                                pr, bpr = psum()
                                fw.op(pe, lambda: nc.tensor.matmul(pr[:, 0:n], RTf, xq[:, 0:n], start=True, stop=True),
                                      reads=[b_cm, bxq], writes=[bpr])
                                lt0 = t0 - C
                                t1, bt1 = f32r.next()
                                fw.op(dve, lambda: nc.vector.tensor_tensor(t1[:, 0:n], pr[:, 0:n], sinT[:, lt0:lt0 + n], ALU.mult),
                                      reads=[bpr, b_cs], writes=[bt1])
                                fw.op(pool, lambda: nc.gpsimd.tensor_tensor(xq[:, 0:n], xq[:, 0:n], cosT[:, lt0:lt0 + n], ALU.mult),
                                      reads=[bxq, b_cs], writes=[bxq])
                                fw.op(dve, lambda: nc.vector.tensor_tensor(ob[:, 0:n], t1[:, 0:n], xq[:, 0:n], ALU.add),
                                      reads=[bt1, bxq], writes=[bob])
                            fw.dma(pool, dst[cc, :, t0:t0 + n], ob[:, 0:n], reads=[bob], writes=[dstb[cc][bi]])
            else:
                for i in range(NT):
                    pp, bpp = psum()
                    for kc in range(KC):
                        fw.op(pe, lambda kc=kc: nc.tensor.matmul(pp, hT[:, kc, i * 128:(i + 1) * 128], sl[:, kc, :],
                                                                 start=(kc == 0), stop=(kc == KC - 1)),
                              reads=[bsl, b_hT[i]], writes=[bpp], inc=(kc == KC - 1))
                    if s == 2:
                        ob, bob = bf16r.next()
                        fw.op(act, lambda: nc.scalar.copy(ob, pp), reads=[bpp], writes=[bob])
                        fw.dma(pool, VS[i * 128:(i + 1) * 128, :], ob, reads=[bob], writes=[VSb[i]])
                    else:
                        of, bof = f32r.next()
                        fw.op(act, lambda: nc.scalar.copy(of, pp), reads=[bpp], writes=[bof])
                        fw.dma(pool, REC[i * 128:(i + 1) * 128, s - 3, :], of, reads=[bof], writes=[RECb[i][s - 3]])

    phase_region()
    kthr = Ring(sbt, "kth", [128, TT], BF16, 2)
    vaugr = Ring(sbt, "vaug", [128, NT, 129], BF16, 2)
    qbr = Ring(sbt, "qb", [128, 256], BF16, 3)
    er = Ring(sbt, "ebuf", [128, 256], BF16, 6)
    o0r = Ring(sbt, "o0", [128, 128], F32, 3)
    yr = Ring(sbt, "yatt", [128, 128], BF16, 3)
    ytr = Ring(sbt, "ytb", [128, 256], BF16, 3)
    smr = Ring(sbt, "sm", [128, 8], F32, 4)
    QB = 256

    def attention(l, need_ctx):
        for va, bva in vaugr.items:
            fw.op(pool, lambda va=va: nc.gpsimd.memset(va[:, :, 128:129], 1.0), writes=[bva])
        for h in range(4):
            kth, bk = kthr.next()
            fw.dma(sp, kth, KT[h], reads=KTb[h], writes=[bk])
            va, bva = vaugr.next()
            vsv = VS.rearrange("(i p) c -> p i c", p=128)
            for i0 in range(0, NT, 6):
                i1 = min(NT, i0 + 6)
                fw.dma(sp, va[:, i0:i1, 0:128], vsv[:, i0:i1, h * 128:(h + 1) * 128], reads=VSb[i0:i1], writes=[bva])
            qblocks = []
            if need_ctx:
                qblocks += [(t0, min(QB, C - t0), NCT) for t0 in range(0, C, QB)]
            qblocks += [(C + t0, min(QB, T - t0), NT) for t0 in range(0, T, QB)]
            for (t0, n, nkt) in qblocks:
                nq = n // 128
                qb, bq = qbr.next()
                fw.dma(sp, qb[:, 0:n], QT[h, :, t0:t0 + n], reads=QTb[h], writes=[bq])
                for kt in range(nkt):
                    for c in range(2):
                        pS, bS = psum(4, 8)
                        fw.op(pe, lambda: nc.tensor.matmul(pS[:, 0:n], kth[64 * c:64 * c + 64, kt * 128:(kt + 1) * 128], qb[64 * c:64 * c + 64, 0:n],
                                                           start=True, stop=True, tile_position=(64 * c, 0)),
                              reads=[bk, bq], writes=[bS])
                        e, be = er.next()
                        fw.op(act, lambda: nc.scalar.activation(e[:, 0:n], pS[:, 0:n], AF.Exp, scale=0.125), reads=[bS], writes=[be])
                        for qt in range(nq):
                            pa, bpa = PS[c * 2 + qt]
                            fw.op(pe, lambda: nc.tensor.matmul(pa[:, 0:129], e[:, qt * 128:(qt + 1) * 128], va[:, kt, :],
                                                               start=(kt == 0), stop=(kt == nkt - 1)),
                                  reads=[be, bva], writes=[bpa], inc=(kt == nkt - 1))
                ytb, bytb = ytr.next()
                for qt in range(nq):
                    p0, bp0 = PS[qt]; p1, bp1 = PS[2 + qt]
                    sm, bsm = smr.next()
                    fw.op(dve, lambda: nc.vector.reciprocal(sm[:, 0:1], p0[:, 128:129]), reads=[bp0], writes=[bsm])
                    fw.op(dve, lambda: nc.vector.reciprocal(sm[:, 1:2], p1[:, 128:129]), reads=[bp1, bsm], writes=[bsm])
                    fw.op(dve, lambda: nc.vector.tensor_tensor(sm[:, 2:3], sm[:, 1:2], lamT[:, 5:6], ALU.mult), reads=[bsm, b_lam], writes=[bsm])
                    o0, bo0 = o0r.next()
                    fw.op(dve, lambda: nc.vector.tensor_scalar(o0, p0[:, 0:128], sm[:, 0:1], None, ALU.mult), reads=[bp0, bsm], writes=[bo0])
                    fw.op(dve, lambda: nc.vector.scalar_tensor_tensor(o0, p1[:, 0:128], sm[:, 2:3], o0, ALU.mult, ALU.add),
                          reads=[bp1, bsm, bo0], writes=[bo0])
                    r, br = rstd_of(o0, bo0, 128, 128)
                    y, by = yr.next()
                    fw.op(dve, lambda: nc.vector.scalar_tensor_tensor(y, o0, r, ANW, ALU.mult, ALU.mult), reads=[bo0, br, b_nw], writes=[by])
                    pT, bpT = psum(4, 8)
                    pTv = pT.bitcast(BF16)
                    fw.op(pe, lambda: nc.tensor.transpose(pTv[:, 0:128], y, identb), reads=[by, b_cb], writes=[bpT])
                    fw.op(act, lambda: nc.scalar.copy(ytb[:, qt * 128:(qt + 1) * 128], pTv[:, 0:128]), reads=[bpT], writes=[bytb])
                fw.dma(pool, YT[h, :, t0:t0 + n], ytb[:, 0:n], reads=[bytb], writes=[YTb[h][t0 // 128 + j] for j in range(nq)])

    phase_region()
    LBt = sbt("LBt", [128, 2, 2, 512], F32)
    lbtmp = sbt("lbtmp", [128, 2, 512], F32)
    glf = Ring(sbt, "glf", [128, 512], F32, 12)
    glb = Ring(sbt, "glb", [128, 512], BF16, 8)
    gtt = Ring(sbt, "gtt", [128, 4, 128], BF16, 6)
    sbr = Ring(sbt, "sbr", [128, 4, 128], BF16, 4)
    Sst = sbt("Sst", [128, 4, 128], F32); b_S = Buf()
    e16r = Ring(sbt, "e16", [128, 4, 4], F32, 3)
    ss4r = Ring(sbt, "ss4", [128, 12], F32, 3)

    def gla(l, need_ctx):
        lb_consts(l)
        for d_ in (1, 0):
            Ld = Lb_f if d_ else Lf_f
            nm = nmask[:, 1, :] if d_ else nmask[:, 0, :]
            zslot = 3 if d_ else 2
            order = list(range(NCT)) + list(range(NCT, NT))
            if d_:
                order = list(range(NCT - 1, -1, -1)) + list(range(NT - 1, NCT - 1, -1))
            fw.op(dve, lambda: nc.vector.memset(Sst.rearrange("p a b -> p (a b)"), 0.0), reads=[b_S], writes=[b_S])
            sb_cur, bsb_cur = sbr.next()
            fw.op(dve, lambda: nc.vector.memset(sb_cur.rearrange("p a b -> p (a b)"), 0.0), writes=[bsb_cur])
            for i in order:
                need_o = need_ctx or i >= NCT
                rows = slice(i * 128, (i + 1) * 128)
                rq, brq = glf.next(); fw.dma(sp, rq, REC[rows, 0, :], reads=[RECb[i][0]], writes=[brq])
                ri, bri = glf.next(); fw.dma(sp, ri, REC[rows, 1, :], reads=[RECb[i][1]], writes=[bri])
                z, bz = glf.next(); fw.dma(sp, z, REC[rows, zslot, :], reads=[RECb[i][zslot]], writes=[bz])
                fw.op(act, lambda: nc.scalar.activation(z, z, AF.Sigmoid), reads=[bz], writes=[bz])
                fw.op(dve, lambda: nc.vector.tensor_tensor(z, z, LBt[:, d_, 1, :], ALU.mult), reads=[bz, b_lb], writes=[bz])
                fw.op(dve, lambda: nc.vector.tensor_tensor(z, z, LBt[:, d_, 0, :], ALU.add), reads=[bz, b_lb], writes=[bz])
                lf, blf = glf.next()
                fw.op(act, lambda: nc.scalar.activation(lf, z, AF.Ln), reads=[bz], writes=[blf])
                pc, bpc = psum(4, 8)
                fw.op(pe, lambda: nc.tensor.matmul(pc, Ld, lf, start=True, stop=True), reads=[b_cm, blf], writes=[bpc])
                pE, bpE = psum(4, 8)
                for h in range(4):
                    fw.op(pe, lambda h=h: nc.tensor.matmul(pE[:, h * 4:(h + 1) * 4], lf[:, h * 128:(h + 1) * 128], indf, start=True, stop=True),
                          reads=[blf, b_cm], writes=[bpE], inc=(h == 3))
                E, bE = e16r.next()
                fw.op(act, lambda: nc.scalar.activation(E.rearrange("p a b -> p (a b)"), pE[:, 0:16], AF.Exp), reads=[bpE], writes=[bE])
                ec, bec = glf.next()
                fw.op(act, lambda: nc.scalar.activation(ec, pc, AF.Exp), reads=[bpc], writes=[bec])
                enc, benc = glf.next()
                fw.op(act, lambda: nc.scalar.activation(enc, pc, AF.Exp, scale=-1.0), reads=[bpc], writes=[benc])
                ktl, bktl = glb.next()
                fw.op(dve, lambda: nc.vector.scalar_tensor_tensor(ktl, z, 1.0, enc, ALU.subtract, ALU.mult), reads=[bz, benc], writes=[bktl])
                vb, bvb = glb.next()
                fw.op(pool, lambda: nc.gpsimd.tensor_copy(vb, ri), reads=[bri], writes=[bvb])
                if need_o:
                    fw.op(act, lambda: nc.scalar.activation(rq, rq, AF.Silu), reads=[brq], writes=[brq])
                    qtl, bqtl = glb.next()
                    fw.op(dve, lambda: nc.vector.tensor_tensor(qtl, rq, ec, ALU.mult), reads=[brq, bec], writes=[bqtl])
                    qtt, bqtt = gtt.next(); transpose_to(qtt, bqtt, qtl, bqtl, 4, 4, 8)
                    ktt, bktt = gtt.next(); transpose_to(ktt, bktt, ktl, bktl, 4, 4, 8)
                    psc, bpsc = psum(4, 8)
                    for h in range(4):
                        fw.op(pe, lambda h=h: nc.tensor.matmul(psc[:, h * 128:(h + 1) * 128], ktt[:, h, :], qtt[:, h, :], start=True, stop=True),
                              reads=[bktt, bqtt], writes=[bpsc], inc=(h == 3))
                    sc, bsc = glb.next()
                    fw.op(dve, lambda: nc.vector.tensor_tensor(sc.rearrange("p (h t) -> p h t", h=4), psc.rearrange("p (h t) -> p h t", h=4),
                                                               nm.unsqueeze(1).to_broadcast([128, 4, 128]), ALU.mult),
                          reads=[bpsc, b_nm], writes=[bsc])
                    for h in range(4):
                        fw.op(pe, lambda h=h: nc.tensor.matmul(PS[h][0][:, 0:128], sc[:, h * 128:(h + 1) * 128], vb[:, h * 128:(h + 1) * 128],
                                                               start=True, stop=False),
                              reads=[bsc, bvb], writes=[PS[h][1]], inc=False)
                corder = (3, 2, 1, 0) if d_ else (0, 1, 2, 3)
                for ci, c in enumerate(corder):
                    cs = slice(32 * c, 32 * c + 32)
                    if need_o:
                        for h in range(4):
                            fw.op(pe, lambda h=h: nc.tensor.matmul(PS[h][0][cs, 0:128], qtt[:, h, cs], sb_cur[:, h, :],
                                                                   start=False, stop=(ci == 3), tile_position=(0, 32 * c)),
                                  reads=[bqtt, bsb_cur], writes=[PS[h][1]], inc=(ci == 3))
                    pP, bpP = psum(4, 8)
                    for h in range(4):
                        fw.op(pe, lambda h=h: nc.tensor.matmul(pP[:, h * 128:(h + 1) * 128], ktl[cs, h * 128:(h + 1) * 128], vb[cs, h * 128:(h + 1) * 128],
                                                               start=True, stop=True, tile_position=(32 * c, 0)),
                              reads=[bktl, bvb], writes=[bpP], inc=(h == 3))
                    S2 = Sst.rearrange("p a b -> p (a b)")
                    fw.op(dve, lambda: nc.vector.tensor_tensor(S2, S2, pP, ALU.subtract), reads=[b_S, bpP], writes=[b_S])
                    fw.op(dve, lambda: nc.vector.tensor_tensor(Sst, Sst, E[:, :, c:c + 1].to_broadcast([128, 4, 128]), ALU.mult),
                          reads=[b_S, bE], writes=[b_S])
                    sb_cur, bsb_cur = sbr.next()
                    fw.op(act, lambda: nc.scalar.copy(sb_cur, Sst), reads=[b_S], writes=[bsb_cur])
                if not need_o:
                    continue
                if d_:
                    of, bof = glf.next()
                    for h in range(4):
                        fw.op(act, lambda h=h: nc.scalar.copy(of[:, h * 128:(h + 1) * 128], PS[h][0][:, 0:128]), reads=[PS[h][1]], writes=[bof])
                    fw.dma(pool, OB[rows, :], of, reads=[bof], writes=[OBb[i]])
                else:
                    obt, bobt = glf.next(); fw.dma(sp, obt, OB[rows, :], reads=[OBb[i]], writes=[bobt])
                    og, bog = glf.next(); fw.dma(sp, og, REC[rows, 4, :], reads=[RECb[i][4]], writes=[bog])
                    for h in range(4):
                        fw.op(dve, lambda h=h: nc.vector.tensor_tensor(obt[:, h * 128:(h + 1) * 128], obt[:, h * 128:(h + 1) * 128], PS[h][0][:, 0:128], ALU.add),
                              reads=[bobt, PS[h][1]], writes=[bobt])
                    s4, bs4 = ss4r.next()
                    jk, bjk = junk.next()
                    fw.op(dve, lambda: nc.vector.memset(s4, 0.0), writes=[bs4])
                    for h in range(4):
                        fw.op(act, lambda h=h: nc.scalar.activation(jk[:, h * 128:(h + 1) * 128], obt[:, h * 128:(h + 1) * 128], AF.Square,
                                                                    accum_out=s4[:, h:h + 1]), reads=[bobt, bs4], writes=[bjk, bs4])
                    fw.op(act, lambda: nc.scalar.activation(s4[:, 4:8], s4[:, 0:4], AF.Sqrt, bias=epsT[:, 0:1], scale=1.0 / 128),
                          reads=[bs4, b_eps], writes=[bs4])
                    fw.op(dve, lambda: nc.vector.reciprocal(s4[:, 8:12], s4[:, 4:8]), reads=[bs4], writes=[bs4])
                    o3 = obt.rearrange("p (h v) -> p h v", h=4)
                    fw.op(dve, lambda: nc.vector.tensor_tensor(o3, o3, s4[:, 8:12].unsqueeze(2).to_broadcast([128, 4, 128]), ALU.mult),
                          reads=[bobt, bs4], writes=[bobt])
                    fw.op(dve, lambda: nc.vector.tensor_tensor(o3, o3, RNW.unsqueeze(1).to_broadcast([128, 4, 128]), ALU.mult),
                          reads=[bobt, b_nw], writes=[bobt])
                    fw.op(act, lambda: nc.scalar.activation(og, og, AF.Silu), reads=[bog], writes=[bog])
                    yb, byb = glb.next()
                    fw.op(dve, lambda: nc.vector.tensor_tensor(yb, obt, og, ALU.mult), reads=[bobt, bog], writes=[byb])
                    ytt, bytt = gtt.next(); transpose_to(ytt, bytt, yb, byb, 4, 4, 8)
                    fw.dma(pool, YT[4:8, :, rows].rearrange("h p t -> p h t"), ytt, reads=[bytt], writes=[YTb[4 + h][i] for h in range(4)])

    phase_region()
    M2 = {k: (sbt(f"m2_{k}", [128, D], F32), Buf(k)) for k in ["g2l", "g2c"]}
    WUA = sbt("WUA", [128, 4, D], BF16); WUR = sbt("WUR", [128, 4, D], BF16); WOUT = sbt("WOUT", [128, KC, D], BF16); b_wm = Buf()
    ytbr = Ring(sbt, "ytblk", [128, 8, 512], BF16, 2)
    gtbr = Ring(sbt, "gtblk", [128, 16, 512], BF16, 2)
    mTr = Ring(sbt, "mT", [128, KC, 512], BF16, 2)

    def merge(l, b, need_ctx):
        load_mod(*M2["g2l"], l, b, 2)
        load_mod(*M2["g2c"], l, NB, 2)
        fw.dma(sp, WUA, wua_b[l].rearrange("(kc p) n -> p kc n", p=128), writes=[b_wm])
        fw.dma(sp, WUR, wur_b[l].rearrange("(kc p) n -> p kc n", p=128), writes=[b_wm])
        fw.dma(sp, WOUT, wout_b[l].rearrange("(kc p) n -> p kc n", p=128), writes=[b_wm])
        for bi, (t0, n) in enumerate(blocks_all):
            if bi == 0 and not need_ctx:
                continue
            tiles = list(range(t0 // 128, (t0 + n) // 128))
            ytb, bytb = ytbr.next()
            fw.dma(sp, ytb[:, :, 0:n], YT[:, :, t0:t0 + n].rearrange("h p t -> p h t"),
                   reads=[YTb[h][i] for h in range(8) for i in tiles], writes=[bytb])
            gtb, bgtb = gtbr.next()
            fw.dma(sp, gtb[:, :, 0:n], GT[:, :, t0:t0 + n].rearrange("h p t -> p h t"), reads=[GTb[g][bi] for g in range(16)], writes=[bgtb])
            mT, bmT = mTr.next()
            for m in range(8):
                pa, bpa = psum(); pb, bpb = psum()
                for kc in range(4):
                    fw.op(pe, lambda kc=kc: nc.tensor.matmul(pa[:, 0:n], WUA[:, kc, m * 128:(m + 1) * 128], ytb[:, kc, 0:n], start=(kc == 0), stop=(kc == 3)),
                          reads=[b_wm, bytb], writes=[bpa], inc=(kc == 3))
                for kc in range(4):
                    fw.op(pe, lambda kc=kc: nc.tensor.matmul(pb[:, 0:n], WUR[:, kc, m * 128:(m + 1) * 128], ytb[:, 4 + kc, 0:n], start=(kc == 0), stop=(kc == 3)),
                          reads=[b_wm, bytb], writes=[bpb], inc=(kc == 3))
                t1, bt1 = f32r.next(); t2, bt2 = f32r.next()
                fw.op(dve, lambda: nc.vector.tensor_tensor(t1[:, 0:n], pa[:, 0:n], gtb[:, m, 0:n], ALU.mult), reads=[bpa, bgtb], writes=[bt1])
                fw.op(dve, lambda: nc.vector.tensor_tensor(t2[:, 0:n], pb[:, 0:n], gtb[:, 8 + m, 0:n], ALU.mult), reads=[bpb, bgtb], writes=[bt2])
                fw.op(pool, lambda: nc.gpsimd.tensor_tensor(mT[:, m, 0:n], t1[:, 0:n], t2[:, 0:n], ALU.add), reads=[bt1, bt2], writes=[bmT])
            suf = "c" if bi == 0 else "l"
            g2, bg2 = M2["g2" + suf]
            for j, i in enumerate(tiles):
                xt, bx = xr.next()
                fw.dma(sp, xt, xsrc(l, b, i), reads=[XSb[i]], writes=[bx])
                xo, bxo = xor_.next()
                for hf in range(2):
                    py, bpy = psum()
                    for kc in range(KC):
                        fw.op(pe, lambda kc=kc: nc.tensor.matmul(py, mT[:, kc, j * 128:(j + 1) * 128], WOUT[:, kc, hf * 512:(hf + 1) * 512],
                                                                 start=(kc == 0), stop=(kc == KC - 1)),
                              reads=[bmT, b_wm], writes=[bpy], inc=(kc == KC - 1))
                    cs = slice(hf * 512, (hf + 1) * 512)
                    fw.op(dve, lambda: nc.vector.tensor_tensor(xo[:, cs], py, g2[:, cs], ALU.mult), reads=[bpy, bg2], writes=[bxo])
                    fw.op(pool, lambda: nc.gpsimd.tensor_tensor(xo[:, cs], xo[:, cs], xt[:, cs], ALU.add), reads=[bxo, bx], writes=[bxo])
                fw.dma(pool, XS[i * 128:(i + 1) * 128, :], xo, reads=[bxo], writes=[XSb[i]])

    NCH_D = (DFF + 127) // 128

    def ffn_set(nblk, dense):
        phase_region()
        F_ = {}
        F_["M"] = {k: (sbt(f"m5_{k}{int(dense)}", [128, D], F32), Buf(k)) for k in ["G2", "S2", "g5"]}
        F_["h2T"] = sbt(f"h2T{int(dense)}", [128, KC, nblk], BF16); F_["b_h2"] = [Buf() for _ in range(nblk // 128)]
        F_["Xblk"] = sbt(f"Xblk{int(dense)}", [128, nblk // 128, D], F32); F_["b_xb"] = [Buf() for _ in range(nblk // 128)]
        F_["aT"] = sbt(f"aT{int(dense)}", [128, 1, 4 if dense else 7, nblk], BF16); F_["b_aT"] = [Buf()]
        F_["w13r"] = Ring(sbt, f"w13{int(dense)}", [128, KC, 512], BF16, 4)
        F_["silr"] = Ring(sbt, f"sil{int(dense)}", [128, 512], F32, 3)
        if dense:
            F_["W2D"] = sbt("W2D", [128, NCH_D, D], BF16); F_["b_w2d"] = Buf()
        else:
            F_["w2r"] = Ring(sbt, "w2q", [128, 7, D], BF16, 2)
            F_["G8"] = sbt("G8", [128, nblk // 128, 8], F32); F_["b_g8"] = Buf()
            F_["rt_sb"] = sbt("rt_sb", [128, KC, NE], BF16); F_["b_rt"] = Buf()
            F_["lgr"] = Ring(sbt, "lg", [128, 32], F32, 3)
            F_["FNW"] = sbt("FNW", [128, D], F32)
        return F_

    FD = ffn_set(512, True) if n_dense else None
    MB = 1024 if T >= 1024 else T
    FM = ffn_set(MB, False) if n_moe else None
    Fc = {}

    def use_set(F_):
        Fc.clear(); Fc.update(F_)

    def load_ffn_mods(l, row):
        M = Fc["M"]
        load_gain_shift(*M["G2"], *M["S2"], l, row, 4, 3, nfw)
        load_mod(*M["g5"], l, row, 5)

    def load_block(l, b, t0, n):
        h2T, Xblk, b_xb, b_h2, M = Fc["h2T"], Fc["Xblk"], Fc["b_xb"], Fc["b_h2"], Fc["M"]
        for j in range(n // 128):
            i = t0 // 128 + j
            fw.dma(sp, Xblk[:, j, :], XS[i * 128:(i + 1) * 128, :], reads=[XSb[i]], writes=[b_xb[j]])
            hb, bhb = hbr.next()
            norm_mod(Xblk[:, j, :], b_xb[j], *M["G2"], *M["S2"], hb, bhb)
            transpose_to(h2T[:, :, j * 128:(j + 1) * 128], b_h2[j], hb, bhb, KC)

    def swiglu_group(w1v, w3v, c0, ncols, n, choff):
        h2T, b_h2, aT, b_aT, w13r, silr = Fc["h2T"], Fc["b_h2"], Fc["aT"], Fc["b_aT"], Fc["w13r"], Fc["silr"]
        s1, bs1 = w13r.next(); s3, bs3 = w13r.next()
        fw.dma(sp, s1[:, :, 0:ncols], w1v[:, :, c0:c0 + ncols], writes=[bs1])
        fw.dma(sp, s3[:, :, 0:ncols], w3v[:, :, c0:c0 + ncols], writes=[bs3])
        nch = (ncols + 127) // 128
        for cc in range(nch):
            w = min(128, ncols - cc * 128)
            for tb in range(0, n, 512):
                nn = min(512, n - tb)
                tl = [b_h2[j] for j in range(tb // 128, (tb + nn) // 128)]
                p1, bp1 = psum(); p3, bp3 = psum()
                for kc in range(KC):
                    fw.op(pe, lambda kc=kc: nc.tensor.matmul(p1[0:w, 0:nn], s1[:, kc, cc * 128:cc * 128 + w], h2T[:, kc, tb:tb + nn],
                                                             start=(kc == 0), stop=(kc == KC - 1)),
                          reads=[bs1] + tl, writes=[bp1], inc=(kc == KC - 1))
                for kc in range(KC):
                    fw.op(pe, lambda kc=kc: nc.tensor.matmul(p3[0:w, 0:nn], s3[:, kc, cc * 128:cc * 128 + w], h2T[:, kc, tb:tb + nn],
                                                             start=(kc == 0), stop=(kc == KC - 1)),
                          reads=[bs3] + tl, writes=[bp3], inc=(kc == KC - 1))
                sl_, bsl_ = silr.next()
                fw.op(act, lambda: nc.scalar.activation(sl_[0:w, 0:nn], p1[0:w, 0:nn], AF.Silu), reads=[bp1], writes=[bsl_])
                fw.op(dve, lambda: nc.vector.tensor_tensor(aT[0:w, 0, choff + cc, tb:tb + nn], sl_[0:w, 0:nn], p3[0:w, 0:nn], ALU.mult),
                      reads=[bsl_, bp3], writes=[b_aT[0]])

    def down_proj(w2t, bw2, ch0, nch, ncols, n, scale_fn):
        aT, b_aT, Xblk, b_xb = Fc["aT"], Fc["b_aT"], Fc["Xblk"], Fc["b_xb"]
        for j in range(n // 128):
            for hf in range(2):
                py, bpy = psum()
                for cc in range(nch):
                    w = min(128, ncols - cc * 128)
                    fw.op(pe, lambda: nc.tensor.matmul(py, aT[0:w, 0, cc, j * 128:(j + 1) * 128], w2t[0:w, ch0 + cc, hf * 512:(hf + 1) * 512],
                                                       start=(cc == 0), stop=(cc == nch - 1)),
                          reads=[b_aT[0], bw2], writes=[bpy], inc=(cc == nch - 1))
                cs = slice(hf * 512, (hf + 1) * 512)
                tq, btq = f32r.next()
                scale_fn(tq, btq, py, bpy, j, cs)
                fw.op(pool, lambda: nc.gpsimd.tensor_tensor(Xblk[:, j, cs], Xblk[:, j, cs], tq, ALU.add), reads=[btq, b_xb[j]], writes=[b_xb[j]])

    def store_block(t0, n):
        Xblk, b_xb = Fc["Xblk"], Fc["b_xb"]
        for j in range(n // 128):
            i = t0 // 128 + j
            fw.dma(pool, XS[i * 128:(i + 1) * 128, :], Xblk[:, j, :], reads=[b_xb[j]], writes=[XSb[i]])

    def ffn_dense(l, b, need_ctx):
        use_set(FD)
        W2D, b_w2d = Fc["W2D"], Fc["b_w2d"]
        j_ = l // 2
        w1v = fw1_b[j_].rearrange("(kc p) f -> p kc f", p=128); w3v = fw3_b[j_].rearrange("(kc p) f -> p kc f", p=128)
        nfull = DFF // 128
        for c0_ in range(0, nfull, 8):
            c1_ = min(nfull, c0_ + 8)
            fw.dma(sp, W2D[:, c0_:c1_, :], fw2_b[j_, c0_ * 128:c1_ * 128, :].rearrange("(ch p) n -> p ch n", p=128), writes=[b_w2d])
        if DFF % 128:
            fw.dma(sp, W2D[0:DFF % 128, nfull, :], fw2_b[j_, nfull * 128:DFF, :], writes=[b_w2d])
        groups = [(c0, min(512, DFF - c0)) for c0 in range(0, DFF, 512)]
        cur_row = None
        for bi, (t0, n) in enumerate(blocks_all):
            if bi == 0 and not need_ctx:
                continue
            row = NB if bi == 0 else b
            if row != cur_row:
                load_ffn_mods(l, row); cur_row = row
            load_block(l, b, t0, n)
            g5, bg5 = Fc["M"]["g5"]

            def sc(tq, btq, py, bpy, j, cs):
                fw.op(dve, lambda: nc.vector.tensor_tensor(tq, py, g5[:, cs], ALU.mult), reads=[bpy, bg5], writes=[btq])
            for (c0, ncols) in groups:
                swiglu_group(w1v, w3v, c0, ncols, n, 0)
                down_proj(W2D, b_w2d, c0 // 128, (ncols + 127) // 128, ncols, n, sc)
            store_block(t0, n)

    def final_norm_store(b, t0, n):
        Xblk, b_xb, FNW = Fc["Xblk"], Fc["b_xb"], Fc["FNW"]
        for j in range(n // 128):
            r, br = rstd_of(Xblk[:, j, :], b_xb[j], D, D)
            xo, bxo = xor_.next()
            fw.op(dve, lambda: nc.vector.scalar_tensor_tensor(xo, Xblk[:, j, :], r, FNW, ALU.mult, ALU.mult), reads=[b_xb[j], br, b_fnw], writes=[bxo])
            lt = t0 - C + j * 128
            out_toks.append(fw.dma(pool, out_d[b, lt:lt + 128, :], xo, reads=[bxo], writes=[Buf()]))

    def ffn_moe(l, b, need_ctx, last):
        use_set(FM)
        h2T, b_h2, G8, b_g8, rt_sb, b_rt, lgr, w2r = Fc["h2T"], Fc["b_h2"], Fc["G8"], Fc["b_g8"], Fc["rt_sb"], Fc["b_rt"], Fc["lgr"], Fc["w2r"]
        j_ = l // 2
        fw.dma(sp, rt_sb, rtw_b[j_].rearrange("(kc p) e -> p kc e", p=128), writes=[b_rt])
        fw.dma(sp, Fc["FNW"], bc(fnw[0:1, :]), writes=[b_fnw])
        mblocks = []
        if need_ctx:
            mblocks.append((0, C, NB))
        mblocks += [(C + t, min(MB, T - t), b) for t in range(0, T, MB)]
        cur_row = None
        for (t0, n, row) in mblocks:
            if row != cur_row:
                load_ffn_mods(l, row); cur_row = row
            load_block(l, b, t0, n)
            g5, bg5 = Fc["M"]["g5"]
            ntl = n // 128
            for j in range(ntl):
                pl, bpl = psum()
                for kc in range(KC):
                    fw.op(pe, lambda kc=kc: nc.tensor.matmul(pl[:, 0:NE], h2T[:, kc, j * 128:(j + 1) * 128], rt_sb[:, kc, :],
                                                             start=(kc == 0), stop=(kc == KC - 1)),
                          reads=[b_h2[j], b_rt], writes=[bpl], inc=(kc == KC - 1))
                lg, blg = lgr.next()
                fw.op(act, lambda: nc.scalar.copy(lg[:, 0:8], pl[:, 0:8]), reads=[bpl], writes=[blg])
                fw.op(dve, lambda: nc.vector.max(lg[:, 8:16], lg[:, 0:8]), reads=[blg], writes=[blg])
                fw.op(dve, lambda: nc.vector.tensor_scalar(lg[:, 16:24], lg[:, 0:8], lg[:, 9:10], None, ALU.is_ge), reads=[blg], writes=[blg])
                fw.op(dve, lambda: nc.vector.tensor_scalar(lg[:, 24:25], lg[:, 8:9], -1.0, None, ALU.mult), reads=[blg], writes=[blg])
                fw.op(act, lambda: nc.scalar.activation(lg[:, 0:8], lg[:, 0:8], AF.Exp, bias=lg[:, 24:25], scale=1.0), reads=[blg], writes=[blg])
                fw.op(dve, lambda: nc.vector.tensor_tensor(lg[:, 0:8], lg[:, 0:8], lg[:, 16:24], ALU.mult), reads=[blg], writes=[blg])
                fw.op(dve, lambda: nc.vector.reduce_sum(lg[:, 25:26], lg[:, 0:8], AX.X), reads=[blg], writes=[blg])
                fw.op(dve, lambda: nc.vector.reciprocal(lg[:, 26:27], lg[:, 25:26]), reads=[blg], writes=[blg])
                fw.op(dve, lambda: nc.vector.tensor_scalar(G8[:, j, :], lg[:, 0:8], lg[:, 26:27], None, ALU.mult), reads=[blg, b_g8], writes=[b_g8])
            for e in range(NE):
                w1v = mw1_b[j_, e].rearrange("(kc p) f -> p kc f", p=128); w3v = mw3_b[j_, e].rearrange("(kc p) f -> p kc f", p=128)
                QW = 896

                def sc(tq, btq, py, bpy, j, cs, e=e):
                    fw.op(dve, lambda: nc.vector.scalar_tensor_tensor(tq, py, G8[:, j, e:e + 1], g5[:, cs], ALU.mult, ALU.mult),
                          reads=[bpy, b_g8, bg5], writes=[btq])
                for c0 in range(0, DFE, QW):
                    ncols = min(QW, DFE - c0)
                    for g0 in range(0, ncols, 512):
                        swiglu_group(w1v, w3v, c0 + g0, min(512, ncols - g0), n, g0 // 128)
                    nch = (ncols + 127) // 128
                    w2q, bw2q = w2r.next()
                    nfull = ncols // 128
                    if nfull:
                        fw.dma(sp, w2q[:, 0:nfull, :], mw2_b[j_, e, c0:c0 + nfull * 128, :].rearrange("(ch p) n -> p ch n", p=128), writes=[bw2q])
                    if ncols % 128:
                        fw.dma(sp, w2q[0:ncols % 128, nfull, :], mw2_b[j_, e, c0 + nfull * 128:c0 + ncols, :], writes=[bw2q])
                    down_proj(w2q, bw2q, 0, nch, ncols, n, sc)
            if last:
                if row != NB:
                    final_norm_store(b, t0, n)
            else:
                store_block(t0, n)

    def barrier():
        fw.barrier()

    stages = []
    done = False
    for b in range(NB):
        for l in range(L):
            last = (l == L - 1)
            need_ctx = not last
            layer_consts(l)
            for name, f in (("p1", lambda: (phase1(l, b), phase1b(l, b))), ("att", lambda: attention(l, need_ctx)),
                            ("gla", lambda: gla(l, need_ctx)), ("merge", lambda: merge(l, b, need_ctx)),
                            ("ffn", lambda: (ffn_dense(l, b, need_ctx) if l % 2 == 0 else ffn_moe(l, b, need_ctx, last)))):
                barrier()
                f()
                if stop_after == (b, l, name):
                    done = True
                    break
            if done:
                break
        if done:
            break
    barrier()
    if dbg:
        for i in range(NT):
            out_toks.append(fw.dma(sp, dbg_d[i * 128:(i + 1) * 128, :], XS[i * 128:(i + 1) * 128, :], reads=[XSb[i]], writes=[Buf()]))
    fw.finish(out_toks, sp)
    fw.finish(out_toks, pool)
    fw.close()
    info = {"n_inst": fw.n_inst, "sbuf_hi": reg["hi"], "sbuf_top": reg["top"]}
    return nc, info


_PROG_CACHE = {}


def _inputs_for_core(inp, b0, NB, consts):
    m = {
        "x": np.ascontiguousarray(inp["x"][b0:b0 + NB]),
        "ctx": np.ascontiguousarray(inp["ctx"][b0:b0 + NB]),
        "cc": np.ascontiguousarray(np.concatenate([inp["c"][b0:b0 + NB], inp["c_ctx"][None, :]], axis=0)),
        "final_norm_w": np.ascontiguousarray(inp["final_norm_w"][None, :]),
    }
    for k in ("w_ada", "b_ada", "norm_mix_w", "norm_ffn_w", "w_in", "lambda_q1", "lambda_k1", "lambda_q2", "lambda_k2",
              "att_norm_w", "rec_norm_w", "lb_fwd", "lb_bwd", "w_up_att", "w_up_rec", "w_out", "ffn_w1", "ffn_w3", "ffn_w2",
              "router_w", "moe_w1", "moe_w3", "moe_w2"):
        m[k] = inp[k]
    m.update(consts)
    return m


def kernel(**inputs):
    inp = {k: np.asarray(v) for k, v in inputs.items()}
    B, T, _ = inp["x"].shape
    C = inp["ctx"].shape[1]
    DFF = inp["ffn_w1"].shape[2]
    DFE = inp["moe_w1"].shape[3]
    ncores = 8
    NB = B // ncores
    key = (NB, T, C, DFF, DFE)
    if key not in _PROG_CACHE:
        _PROG_CACHE[key] = build_program(NB, T, C, DFF, DFE)[0]
    nc = _PROG_CACHE[key]
    consts = host_consts(T)
    in_maps = [_inputs_for_core(inp, c * NB, NB, consts) for c in range(ncores)]
    res = run_bass_kernel_spmd(nc, in_maps, core_ids=list(range(ncores)))
    return np.concatenate([np.asarray(r["out"]) for r in res.results], axis=0).astype(np.float32)
```

```python
import math
import numpy as np
import ml_dtypes
import concourse.bass as bass
import concourse.mybir as mybir
from concourse.bass_utils import run_bass_kernel_spmd

F32 = mybir.dt.float32
BF16 = mybir.dt.bfloat16
AF = mybir.ActivationFunctionType
ALU = mybir.AluOpType
AX = mybir.AxisListType


class Buf:
    __slots__ = ("name", "w", "readers")

    def __init__(self, name=""):
        self.name = name
        self.w = None
        self.readers = []


class Eng:
    def __init__(self, fw, name, handle, compute=True):
        self.fw = fw
        self.name = name
        self.h = handle
        self.compute = compute
        self.sem = None
        self.count = 0
        self.known = {}
        self.nsem = 0
        self.pending = False
        if compute:
            self._new_sem()

    def _new_sem(self):
        self.sem = self.fw.new_sem(f"{self.name}{self.nsem}")
        self.nsem += 1
        self.count = 0

    def wait(self, tok):
        sem, val, eng = tok
        k = id(sem)
        if self.known.get(k, 0) >= val:
            return
        if eng is self and eng.compute and sem is eng.sem and val > eng.count:
            raise RuntimeError(f"{self.name}: waiting on own future token")
        self.h.wait_ge(sem, val)
        self.known[k] = val


class FW:
    ROLL = 30000

    def __init__(self, nc):
        self.nc = nc
        self._stack = []
        self._sems = []
        self.pe = Eng(self, "pe", nc.tensor)
        self.act = Eng(self, "act", nc.scalar)
        self.dve = Eng(self, "dve", nc.vector)
        self.pool = Eng(self, "pool", nc.gpsimd)
        self.sp = Eng(self, "sp", nc.sync)
        self.engs = [self.pe, self.act, self.dve, self.pool, self.sp]
        self.dma_rings = {}
        self.n_inst = 0

    def new_sem(self, name):
        cm = self.nc.semaphore(name)
        s = cm.__enter__()
        self._stack.append(cm)
        self._sems.append(s)
        return s

    def close(self):
        for cm in reversed(self._stack):
            cm.__exit__(None, None, None)
        self._stack = []

    def op(self, eng, fn, reads=(), writes=(), inc=True):
        toks = {}
        for b in reads:
            if b.w is not None:
                toks[(id(b.w[0]), b.w[1])] = b.w
        for b in writes:
            if b.w is not None:
                toks[(id(b.w[0]), b.w[1])] = b.w
            for r in b.readers:
                toks[(id(r[0]), r[1])] = r
        rtoks = set()
        for b_ in reads:
            if b_.w is not None:
                rtoks.add((id(b_.w[0]), b_.w[1]))
        for k_, t in toks.items():
            if t[2] is eng and eng is self.pe:
                continue
            if t[2] is eng and k_ not in rtoks:
                continue
            if t[2] is eng and t[0] is eng.sem and t[1] > eng.count:
                continue
            eng.wait(t)
        inst = fn()
        self.n_inst += 1
        if inc:
            if eng.count >= self.ROLL and not eng.pending:
                eng._new_sem()
            eng.count += 1
            eng.pending = False
            inst.then_inc(eng.sem, 1)
            tok = (eng.sem, eng.count, eng)
        else:
            eng.pending = True
            tok = (eng.sem, eng.count + 1, eng)
        for b in reads:
            b.readers = [r for r in b.readers if r[2] is not eng or r[0] is not tok[0]]
            b.readers.append(tok)
        for b in writes:
            b.w = tok
            b.readers = []
        return inst

    def dma(self, q, out, in_, reads=(), writes=(), ring=None, **kw):
        rname = ring or q.name
        ring = self.dma_rings.get(rname)
        if ring is None:
            nr = 12
            ring = {"sems": [self.new_sem(f"d{rname}{i}") for i in range(nr)],
                    "cnt": [0] * nr, "i": 0}
            self.dma_rings[rname] = ring
        i = ring["i"]
        ring["i"] = (i + 1) % len(ring["sems"])
        sem = ring["sems"][i]
        toks = {}
        for b in reads:
            if b.w is not None:
                toks[(id(b.w[0]), b.w[1])] = b.w
        for b in writes:
            if b.w is not None:
                toks[(id(b.w[0]), b.w[1])] = b.w
            for r in b.readers:
                toks[(id(r[0]), r[1])] = r
        if ring["cnt"][i] > 0:
            toks[(id(sem), ring["cnt"][i])] = (sem, ring["cnt"][i], None)
        for t in toks.values():
            q.wait(t)
        if ring["cnt"][i] + 16 > self.ROLL:
            sem = self.new_sem(f"d{q.name}{i}r{self.n_inst}")
            ring["sems"][i] = sem
            ring["cnt"][i] = 0
        inst = q.h.dma_start(out=out, in_=in_, **kw)
        self.n_inst += 1
        ring["cnt"][i] += 16
        inst.then_inc(sem, 16)
        tok = (sem, ring["cnt"][i], None)
        for b in reads:
            b.readers.append(tok)
        for b in writes:
            b.w = tok
            b.readers = []
        return tok

    def barrier(self):
        toks = []
        for e in self.engs:
            if e.count > 0:
                toks.append((e.sem, e.count, e))
        for rname, ring in self.dma_rings.items():
            if rname == "conv":
                continue
            for sem, cnt in zip(ring["sems"], ring["cnt"]):
                if cnt > 0:
                    toks.append((sem, cnt, None))
        for e in self.engs:
            for t in toks:
                if t[2] is e:
                    continue
                e.wait(t)

    def finish(self, toks, eng=None):
        eng = eng or self.sp
        for t in toks:
            eng.wait(t)


class Ring:
    def __init__(self, alloc, name, shape, dtype, n):
        self.items = [(alloc(f"{name}{i}", shape, dtype), Buf(f"{name}{i}")) for i in range(n)]
        self.i = 0

    def next(self):
        it = self.items[self.i]
        self.i = (self.i + 1) % len(self.items)
        return it


D = 1024
KC = 8
DIN = 6144
NE = 8
EPS = 1e-6
CH = 32
GRID_W = 64


def host_consts(T):
    c = {}
    ident = np.eye(128, dtype=np.float32)
    s = np.arange(128)
    same = (s[:, None] // CH) == (s[None, :] // CH)
    Lf = (same & (s[:, None] <= s[None, :])).astype(np.float32)
    Lb = (same & (s[:, None] >= s[None, :])).astype(np.float32)
    ind = (s[:, None] // CH == np.arange(4)[None, :]).astype(np.float32)
    RT = np.zeros((128, 128), np.float32)
    for m in range(128):
        if m % 64 < 32:
            RT[m + 32, m] = -1.0
        else:
            RT[m - 32, m] = 1.0
    c["cmat"] = np.concatenate([ident, Lf, Lb, RT, ind, np.ones((128, 4), np.float32)], axis=1)
    rows = T // GRID_W
    row = np.repeat(np.arange(rows, dtype=np.float32), GRID_W)
    col = np.tile(np.arange(GRID_W, dtype=np.float32), rows)
    nf = 16
    inv = (10000.0 ** (-np.arange(nf, dtype=np.float32) / nf)).astype(np.float32)
    ang = np.concatenate([row[:, None] * inv, col[:, None] * inv], axis=-1).astype(np.float32)
    cosT = np.cos(ang).astype(np.float32).T
    sinT = np.sin(ang).astype(np.float32).T
    c["cos"] = np.ascontiguousarray(np.tile(cosT, (4, 1)))
    c["sin"] = np.ascontiguousarray(np.tile(sinT, (4, 1)))
    return c


def build_program(NB, T, C, DFF, DFE, depth=2, stop_after=None, dbg=False):
    nc = bass.Bass("TRN2", target_bir_lowering=False)
    fw = FW(nc)
    pe, act, dve, pool, sp = fw.pe, fw.act, fw.dve, fw.pool, fw.sp
    TT = C + T
    NT = TT // 128
    NCT = C // 128
    NLT = T // 128
    L = depth
    n_dense = (L + 1) // 2
    n_moe = L // 2

    def din(name, shape, dt=F32):
        return nc.dram_tensor(name, list(shape), dt, kind="ExternalInput").ap()

    def dscr(name, shape, dt):
        if dbg and name in ("mods_s", "XS", "QT", "KT", "VS", "REC", "GT", "YT", "OB"):
            return nc.dram_tensor(name, list(shape), dt, kind="ExternalOutput").ap()
        return nc.dram_tensor(name, list(shape), dt).ap()

    x_d = din("x", [NB, T, D]); ctx_d = din("ctx", [NB, C, D]); cc_d = din("cc", [NB + 1, D])
    w_ada = din("w_ada", [L, D, 6 * D]); b_ada = din("b_ada", [L, 6 * D])
    nmw = din("norm_mix_w", [L, D]); nfw = din("norm_ffn_w", [L, D])
    w_in = din("w_in", [L, D, DIN])
    lq1 = din("lambda_q1", [L, 64]); lk1 = din("lambda_k1", [L, 64]); lq2 = din("lambda_q2", [L, 64]); lk2 = din("lambda_k2", [L, 64])
    anw = din("att_norm_w", [L, 128]); rnw = din("rec_norm_w", [L, 128])
    lbf = din("lb_fwd", [L, 512]); lbb = din("lb_bwd", [L, 512])
    wua = din("w_up_att", [L, 512, D]); wur = din("w_up_rec", [L, 512, D]); wout = din("w_out", [L, D, D])
    fw1 = din("ffn_w1", [n_dense, D, DFF]); fw3 = din("ffn_w3", [n_dense, D, DFF]); fw2 = din("ffn_w2", [n_dense, DFF, D])
    rtw = din("router_w", [n_moe, D, NE])
    mw1 = din("moe_w1", [n_moe, NE, D, DFE]); mw3 = din("moe_w3", [n_moe, NE, D, DFE]); mw2 = din("moe_w2", [n_moe, NE, DFE, D])
    fnw = din("final_norm_w", [1, D])
    cmat_d = din("cmat", [128, 520]); cos_d = din("cos", [128, T]); sin_d = din("sin", [128, T])
    out_d = nc.dram_tensor("out", [NB, T, D], F32, kind="ExternalOutput").ap()
    dbg_d = nc.dram_tensor("dbg", [TT, D], F32, kind="ExternalOutput").ap() if dbg else None

    w_in_b = dscr("w_in_b", [L, D, DIN], BF16)
    wua_b = dscr("wua_b", [L, 512, D], BF16); wur_b = dscr("wur_b", [L, 512, D], BF16); wout_b = dscr("wout_b", [L, D, D], BF16)
    fw1_b = dscr("fw1_b", [n_dense, D, DFF], BF16); fw3_b = dscr("fw3_b", [n_dense, D, DFF], BF16); fw2_b = dscr("fw2_b", [n_dense, DFF, D], BF16)
    mw1_b = dscr("mw1_b", [n_moe, NE, D, DFE], BF16); mw3_b = dscr("mw3_b", [n_moe, NE, D, DFE], BF16); mw2_b = dscr("mw2_b", [n_moe, NE, DFE, D], BF16)
    rtw_b = dscr("rtw_b", [n_moe, D, NE], BF16)
    mods_s = dscr("mods_s", [L, NB + 1, 6 * D], F32)
    XS = dscr("XS", [TT, D], F32)
    QT = dscr("QT", [4, 128, TT], BF16); KT = dscr("KT", [4, 128, TT], BF16)
    VS = dscr("VS", [TT, 512], BF16)
    REC = dscr("REC", [TT, 5, 512], F32)
    GT = dscr("GT", [16, 128, TT], BF16)
    YT = dscr("YT", [8, 128, TT], BF16)
    OB = dscr("OB", [TT, 512], F32)

    ARENA = 52000
    arena = nc.alloc_sbuf_tensor("arena", [128, ARENA], F32).ap()
    reg = {"cur": 0, "hi": 0, "top": ARENA, "persist": True}

    def sbt(name, shape, dt):
        shape = list(shape)
        n = int(np.prod(shape[1:]))
        w = (n + 1) // 2 if dt == BF16 else n
        w = (w + 7) // 8 * 8
        if reg["persist"]:
            reg["top"] -= w
            o = reg["top"]
        else:
            o = reg["cur"]
            reg["cur"] = o + w
            reg["hi"] = max(reg["hi"], reg["cur"])
        assert reg["hi"] <= reg["top"], f"SBUF arena overflow at {name}: {reg['hi']} > {reg['top']}"
        v = arena[0:shape[0], o:o + ((n + 1) // 2 if dt == BF16 else n)]
        if dt == BF16:
            v = v.bitcast(BF16)[:, 0:n]
        if len(shape) == 3:
            v = v.rearrange("p (a b) -> p a b", a=shape[1])
        elif len(shape) == 4:
            v = v.rearrange("p (a b c) -> p a b c", a=shape[1], b=shape[2])
        return v

    def phase_region():
        reg["cur"] = 0
        reg["persist"] = False

    def persist_region():
        reg["persist"] = True
    pst = lambda n, s, d: nc.alloc_psum_tensor(n, list(s), d).ap()

    PS = [(pst(f"ps{i}", [128, 512], F32), Buf(f"ps{i}")) for i in range(8)]
    ps_rr = [0]

    def psum(lo=0, hi=8):
        i = ps_rr[0] % (hi - lo) + lo
        ps_rr[0] += 1
        return PS[i]

    out_toks = []

    cmat = sbt("cmat", [128, 520], F32); b_cm = Buf()
    fw.dma(sp, cmat, cmat_d, writes=[b_cm])
    identf = cmat[:, 0:128]; Lf_f = cmat[:, 128:256]; Lb_f = cmat[:, 256:384]; RTf = cmat[:, 384:512]; indf = cmat[:, 512:516]
    cbf = sbt("cbf", [128, 520], BF16); b_cb = Buf()
    fw.op(dve, lambda: nc.vector.tensor_copy(cbf, cmat), reads=[b_cm], writes=[b_cb])
    identb = cbf[:, 0:128]
    nmask = sbt("nmask", [128, 2, 128], F32); b_nm = Buf()
    fw.op(dve, lambda: nc.vector.tensor_scalar(nmask.rearrange("p a b -> p (a b)"), cmat[:, 128:384], -1.0, None, ALU.mult),
          reads=[b_cm], writes=[b_nm])
    epsT = sbt("epsT", [128, 1], F32); b_eps = Buf()
    fw.op(pool, lambda: nc.gpsimd.memset(epsT, EPS), writes=[b_eps])

    wb = Buf("weights_b")

    def flat2(ap, cols):
        nd = len(ap.shape)
        names = " ".join(f"d{i}" for i in range(nd))
        f = ap.rearrange(f"{names} -> ({names})")
        return f.rearrange("(r c) -> r c", c=cols)

    conv_list = [(w_in_b, w_in), (wua_b, wua), (wur_b, wur), (wout_b, wout), (fw1_b, fw1), (fw3_b, fw3), (fw2_b, fw2),
                 (mw1_b, mw1), (mw3_b, mw3), (mw2_b, mw2), (rtw_b, rtw)]
    conv_early = []
    conv_late = []
    conv_late_toks = []
    for dst, src in conv_list:
        n = int(np.prod(dst.shape))
        if n == 0:
            continue
        cols = 2048
        while n % cols:
            cols //= 2
        d2 = flat2(dst, cols); s2 = flat2(src, cols)
        rows = n // cols
        step = 2048
        late = dst is mw1_b or dst is mw3_b or dst is mw2_b
        for r0 in range(0, rows, step):
            r1 = min(rows, r0 + step)
            if late:
                conv_late.append((d2[r0:r1, :], s2[r0:r1, :]))
            else:
                conv_early.append(fw.dma(pool, d2[r0:r1, :], s2[r0:r1, :], writes=[Buf()], ring="conv"))
    for t in conv_early:
        sp.wait(t)

    def inject_conv(k):
        for _ in range(k):
            if conv_late:
                d2_, s2_ = conv_late.pop(0)
                conv_late_toks.append(fw.dma(pool, d2_, s2_, writes=[Buf()], ring="conv"))

    def need_moe_weights():
        inject_conv(len(conv_late))
        for t in conv_late_toks:
            sp.wait(t)

    NR = NB + 1
    ones_row = sbt("ones_row", [1, 8], F32); b_or = Buf()
    phase_region()
    ccs = sbt("ccs", [NR, D], F32); b_cc = Buf()
    fw.dma(sp, ccs, cc_d, writes=[b_cc])
    fw.op(act, lambda: nc.scalar.activation(ccs, ccs, AF.Silu), reads=[b_cc], writes=[b_cc])
    scT = sbt("scT", [128, KC, NR], F32); b_scT = Buf()
    pt, bpt = psum()
    for kc in range(KC):
        fw.op(pe, lambda kc=kc: nc.tensor.transpose(pt[:, kc * NR:(kc + 1) * NR], ccs[:, kc * 128:(kc + 1) * 128], identf[0:NR, 0:NR]),
              reads=[b_cc, b_cm], writes=[bpt], inc=(kc == KC - 1))
    fw.op(act, lambda: nc.scalar.copy(scT.rearrange("p a b -> p (a b)"), pt[:, 0:KC * NR]), reads=[bpt], writes=[b_scT])
    onesr = cmat[0:1, 516:516 + 1]
    fw.op(pool, lambda: nc.gpsimd.memset(ones_row, 1.0), writes=[b_or])
    adar = Ring(sbt, "adaw", [128, KC, 512], F32, 2)
    bar = Ring(sbt, "adab", [1, 512], F32, 2)
    mor = Ring(sbt, "adao", [NR, 512], F32, 2)
    b_mods = Buf("mods")
    mods_toks = []
    for l in range(L):
        for j in range(12):
            wsl, bw = adar.next()
            fw.dma(sp, wsl, w_ada[l].rearrange("(kc p) n -> p kc n", p=128)[:, :, j * 512:(j + 1) * 512], writes=[bw])
            bsl, bb = bar.next()
            fw.dma(sp, bsl, b_ada[l:l + 1, j * 512:(j + 1) * 512], writes=[bb])
            pm, bpm = psum()
            for kc in range(KC):
                fw.op(pe, lambda kc=kc: nc.tensor.matmul(pm[0:NR, :], scT[:, kc, :], wsl[:, kc, :], start=(kc == 0), stop=False),
                      reads=[b_scT, bw], writes=[bpm], inc=False)
            fw.op(pe, lambda: nc.tensor.matmul(pm[0:NR, :], ones_row[0:1, 0:NR], bsl, start=False, stop=True),
                  reads=[b_or, bb], writes=[bpm])
            mo, bmo = mor.next()
            fw.op(act, lambda: nc.scalar.copy(mo, pm[0:NR, :]), reads=[bpm], writes=[bmo])
            mods_toks.append(fw.dma(sp, mods_s[l, :, j * 512:(j + 1) * 512], mo, reads=[bmo], writes=[Buf()]))
    for t in mods_toks:
        sp.wait(t)

    bc = lambda ap_row: ap_row.partition_broadcast(128)

    persist_region()
    lamT = sbt("lamT", [128, 8], F32); b_lam = Buf()
    lqk = sbt("lqk", [128, 4, 64], F32); b_lqk = Buf()
    ANW = sbt("ANW", [128, 128], F32); RNW = sbt("RNW", [128, 128], F32); b_nw = Buf()
    b_lb = Buf(); b_lbt = Buf(); b_fnw = Buf()

    def layer_consts(l):
        lam_init = 0.8 - 0.6 * math.exp(-0.3 * l)
        for i, a in enumerate([lq1, lk1, lq2, lk2]):
            fw.dma(sp, lqk[:, i, :], bc(a[l:l + 1, :]), writes=[b_lqk])
        fw.op(dve, lambda: nc.vector.tensor_tensor(lqk[:, 0, :], lqk[:, 0, :], lqk[:, 1, :], ALU.mult), reads=[b_lqk], writes=[b_lqk])
        fw.op(dve, lambda: nc.vector.tensor_tensor(lqk[:, 2, :], lqk[:, 2, :], lqk[:, 3, :], ALU.mult), reads=[b_lqk], writes=[b_lqk])
        fw.op(dve, lambda: nc.vector.reduce_sum(lamT[:, 0:1], lqk[:, 0, :], AX.X), reads=[b_lqk, b_lam], writes=[b_lam])
        fw.op(dve, lambda: nc.vector.reduce_sum(lamT[:, 1:2], lqk[:, 2, :], AX.X), reads=[b_lqk, b_lam], writes=[b_lam])
        fw.op(act, lambda: nc.scalar.activation(lamT[:, 2:4], lamT[:, 0:2], AF.Exp), reads=[b_lam], writes=[b_lam])
        fw.op(dve, lambda: nc.vector.tensor_tensor(lamT[:, 4:5], lamT[:, 2:3], lamT[:, 3:4], ALU.subtract), reads=[b_lam], writes=[b_lam])
        fw.op(dve, lambda: nc.vector.tensor_scalar(lamT[:, 5:6], lamT[:, 4:5], lam_init, -1.0, ALU.add, ALU.mult), reads=[b_lam], writes=[b_lam])
        fw.dma(sp, ANW, bc(anw[l:l + 1, :]), writes=[b_nw])
        fw.dma(sp, RNW, bc(rnw[l:l + 1, :]), writes=[b_nw])
        fw.op(dve, lambda: nc.vector.tensor_scalar(ANW, ANW, 1.0 - lam_init, None, ALU.mult), reads=[b_nw], writes=[b_nw])
    def lb_consts(l):
        for d_, a in enumerate([lbf, lbb]):
            if l == 0:
                fw.op(dve, lambda d_=d_: nc.vector.memset(LBt[:, d_, 0, :], 0.0), writes=[b_lb])
                fw.op(dve, lambda d_=d_: nc.vector.memset(LBt[:, d_, 1, :], 1.0), writes=[b_lb])
            else:
                assert L == 2 and l == 1
                fw.dma(sp, lbtmp[:, 0, :], bc(a[0:1, :]), writes=[b_lbt])
                fw.dma(sp, lbtmp[:, 1, :], bc(a[1:2, :]), writes=[b_lbt])
                fw.op(dve, lambda: nc.vector.tensor_tensor(lbtmp[:, 0, :], lbtmp[:, 1, :], lbtmp[:, 0, :], ALU.subtract), reads=[b_lbt], writes=[b_lbt])
                fw.op(act, lambda d_=d_: nc.scalar.activation(LBt[:, d_, 0, :], lbtmp[:, 0, :], AF.Sigmoid), reads=[b_lbt, b_lb], writes=[b_lb])
                fw.op(dve, lambda d_=d_: nc.vector.tensor_scalar(LBt[:, d_, 1, :], LBt[:, d_, 0, :], -1.0, 1.0, ALU.mult, ALU.add), reads=[b_lb], writes=[b_lb])

    def load_mod(t, bt, l, row, j):
        fw.dma(sp, t, bc(mods_s[l, row:row + 1, j * D:(j + 1) * D]), writes=[bt])

    def load_gain_shift(G, bG, S, bS, l, row, jscale, jshift, nw):
        fw.dma(sp, S, bc(nw[l:l + 1, :]), writes=[bS])
        load_mod(G, bG, l, row, jscale)
        fw.op(dve, lambda: nc.vector.scalar_tensor_tensor(G, G, 1.0, S, ALU.add, ALU.mult), reads=[bG, bS], writes=[bG])
        load_mod(S, bS, l, row, jshift)

    ssr = Ring(sbt, "ss", [128, 4], F32, 4)
    junk = Ring(sbt, "junk", [128, D], BF16, 2)
    tmpr = Ring(sbt, "nrm_t", [128, D], F32, 2)

    def rstd_of(xt, bx, n, width):
        ss, bss = ssr.next()
        jk, bjk = junk.next()
        fw.op(dve, lambda: nc.vector.memset(ss, 0.0), writes=[bss])
        fw.op(act, lambda: nc.scalar.activation(jk[:, 0:width], xt, AF.Square, accum_out=ss[:, 0:1]), reads=[bx, bss], writes=[bjk, bss])
        fw.op(act, lambda: nc.scalar.activation(ss[:, 1:2], ss[:, 0:1], AF.Sqrt, bias=epsT[:, 0:1], scale=1.0 / n), reads=[bss, b_eps], writes=[bss])
        fw.op(dve, lambda: nc.vector.reciprocal(ss[:, 2:3], ss[:, 1:2]), reads=[bss], writes=[bss])
        return ss[:, 2:3], bss

    def norm_mod(xt, bx, G, bG, S, bS, hb, bhb):
        r, br = rstd_of(xt, bx, D, D)
        t, bt = tmpr.next()
        fw.op(dve, lambda: nc.vector.scalar_tensor_tensor(t, xt, r, G, ALU.mult, ALU.mult), reads=[bx, br, bG], writes=[bt])
        fw.op(pool, lambda: nc.gpsimd.tensor_tensor(hb, t, S, ALU.add), reads=[bt, bS], writes=[bhb])

    xr = Ring(sbt, "xt", [128, D], F32, 2)
    hbr = Ring(sbt, "hb", [128, D], BF16, 2)
    xor_ = Ring(sbt, "xo", [128, D], F32, 2)
    f32r = Ring(sbt, "f32t", [128, 512], F32, 4)
    bf16r = Ring(sbt, "bf16t", [128, 512], BF16, 4)
    phase_region()
    hT = sbt("hT", [128, KC, TT], BF16); b_hT = [Buf(f"hT{i}") for i in range(NT)]
    cosT = sbt("cosT", [128, T], F32); sinT = sbt("sinT", [128, T], F32); b_cs = Buf()
    slabr = Ring(sbt, "wslab", [128, KC, 512], BF16, 2)
    M1 = {k: (sbt(f"m1_{k}", [128, D], F32), Buf(k)) for k in ["G1l", "S1l", "G1c", "S1c"]}
    XSb = [Buf(f"XS{i}") for i in range(NT)]

    def xsrc(l, b, i):
        if l == 0:
            if i < NCT:
                return ctx_d[b, i * 128:(i + 1) * 128, :]
            return x_d[b, (i - NCT) * 128:(i - NCT + 1) * 128, :]
        return XS[i * 128:(i + 1) * 128, :]

    def transpose_to(dst3, bdst, src, bsrc, nchunk, lo=0, hi=8):
        ptt, bptt = psum(lo, hi)
        pv = ptt.bitcast(BF16)
        for kc in range(nchunk):
            fw.op(pe, lambda kc=kc: nc.tensor.transpose(pv[:, kc * 128:(kc + 1) * 128], src[:, kc * 128:(kc + 1) * 128], identb),
                  reads=[bsrc, b_cb], writes=[bptt], inc=(kc == nchunk - 1))
        fw.op(act, lambda: nc.scalar.copy(dst3, pv[:, 0:nchunk * 128].rearrange("p (k t) -> p k t", k=nchunk)),
              reads=[bptt], writes=[bdst])

    blocks_all = [(0, C)] + [(C + i * 512, min(512, TT - C - i * 512)) for i in range((T + 511) // 512)]
    QTb = [[Buf() for _ in blocks_all] for _ in range(4)]
    KTb = [[Buf() for _ in blocks_all] for _ in range(4)]
    VSb = [Buf() for _ in range(NT)]
    RECb = [[Buf() for _ in range(5)] for _ in range(NT)]
    GTb = [[Buf() for _ in blocks_all] for _ in range(16)]
    YTb = [[Buf() for _ in range(NT)] for _ in range(8)]
    OBb = [Buf() for _ in range(NT)]

    def phase1(l, b):
        load_gain_shift(*M1["G1l"], *M1["S1l"], l, b, 1, 0, nmw)
        load_gain_shift(*M1["G1c"], *M1["S1c"], l, NB, 1, 0, nmw)
        for i in range(NT):
            xt, bx = xr.next()
            src_b = XSb[i] if l > 0 else Buf()
            fw.dma(sp, xt, xsrc(l, b, i), reads=[src_b], writes=[bx])
            suf = "c" if i < NCT else "l"
            hb, bhb = hbr.next()
            norm_mod(xt, bx, *M1["G1" + suf], *M1["S1" + suf], hb, bhb)
            transpose_to(hT[:, :, i * 128:(i + 1) * 128], b_hT[i], hb, bhb, KC)

    def phase1b(l, b):
        wv = w_in_b[l].rearrange("(kc p) n -> p kc n", p=128)
        fw.dma(sp, cosT, cos_d, writes=[b_cs]); fw.dma(sp, sinT, sin_d, writes=[b_cs])
        for s in range(12):
            sl, bsl = slabr.next()
            fw.dma(sp, sl, wv[:, :, s * 512:(s + 1) * 512], writes=[bsl])
            if s in (0, 1, 8, 9, 10, 11):
                for cc in range(4):
                    for bi, (t0, n) in enumerate(blocks_all):
                        tiles = range(t0 // 128, (t0 + n) // 128)
                        pp, bpp = psum()
                        for kc in range(KC):
                            fw.op(pe, lambda kc=kc: nc.tensor.matmul(pp[:, 0:n], sl[:, kc, cc * 128:(cc + 1) * 128], hT[:, kc, t0:t0 + n],
                                                                     start=(kc == 0), stop=(kc == KC - 1)),
                                  reads=[bsl] + [b_hT[i] for i in tiles], writes=[bpp], inc=(kc == KC - 1))
                        ob, bob = bf16r.next()
                        if s >= 8:
                            g = (s - 8) * 4 + cc
                            fw.op(act, lambda: nc.scalar.activation(ob[:, 0:n], pp[:, 0:n], AF.Sigmoid), reads=[bpp], writes=[bob])
                            fw.dma(pool, GT[g, :, t0:t0 + n], ob[:, 0:n], reads=[bob], writes=[GTb[g][bi]])
                        else:
                            dst, dstb = (QT, QTb) if s == 0 else (KT, KTb)
                            if bi == 0:
                                fw.op(act, lambda: nc.scalar.copy(ob[:, 0:n], pp[:, 0:n]), reads=[bpp], writes=[bob])
                            else:
                                xq, bxq = f32r.next()
                                fw.op(act, lambda: nc.scalar.copy(xq[:, 0:n], pp[:, 0:n]), reads=[bpp], writes=[bxq])
                                pr, bpr = psum()
                                fw.op(pe, lambda: nc.tensor.matmul(pr[:, 0:n], RTf, xq[:, 0:n], start=True, stop=True),
                                      reads=[b_cm, bxq], writes=[bpr])
                                lt0 = t0 - C
                                t1, bt1 = f32r.next()
                                fw.op(dve, lambda: nc.vector.tensor_tensor(t1[:, 0:n], pr[:, 0:n], sinT[:, lt0:lt0 + n], ALU.mult),
                                      reads=[bpr, b_cs], writes=[bt1])
                                fw.op(pool, lambda: nc.gpsimd.tensor_tensor(xq[:, 0:n], xq[:, 0:n], cosT[:, lt0:lt0 + n], ALU.mult),
                                      reads=[bxq, b_cs], writes=[bxq])
                                fw.op(dve, lambda: nc.vector.tensor_tensor(ob[:, 0:n], t1[:, 0:n], xq[:, 0:n], ALU.add),
                                      reads=[bt1, bxq], writes=[bob])
                            fw.dma(pool, dst[cc, :, t0:t0 + n], ob[:, 0:n], reads=[bob], writes=[dstb[cc][bi]])
            else:
                for i in range(NT):
                    pp, bpp = psum()
                    for kc in range(KC):
                        fw.op(pe, lambda kc=kc: nc.tensor.matmul(pp, hT[:, kc, i * 128:(i + 1) * 128], sl[:, kc, :],
                                                                 start=(kc == 0), stop=(kc == KC - 1)),
                              reads=[bsl, b_hT[i]], writes=[bpp], inc=(kc == KC - 1))
                    if s == 2:
                        ob, bob = bf16r.next()
                        fw.op(act, lambda: nc.scalar.copy(ob, pp), reads=[bpp], writes=[bob])
                        fw.dma(pool, VS[i * 128:(i + 1) * 128, :], ob, reads=[bob], writes=[VSb[i]])
                    else:
                        of, bof = f32r.next()
                        fw.op(act, lambda: nc.scalar.copy(of, pp), reads=[bpp], writes=[bof])
                        fw.dma(pool, REC[i * 128:(i + 1) * 128, s - 3, :], of, reads=[bof], writes=[RECb[i][s - 3]])

    phase_region()
    kthr = Ring(sbt, "kth", [128, TT], BF16, 2)
    vaugr = Ring(sbt, "vaug", [128, NT, 129], BF16, 2)
    qbr = Ring(sbt, "qb", [128, 512], BF16, 2)
    er = Ring(sbt, "ebuf", [128, 512], BF16, 6)
    o0r = Ring(sbt, "o0", [128, 4, 128], F32, 2)
    yr = Ring(sbt, "yatt", [128, 128], BF16, 3)
    ytr = Ring(sbt, "ytb", [128, 512], BF16, 2)
    smr = Ring(sbt, "sm", [128, 8], F32, 6)
    QB = 512

    def attention(l, need_ctx):
        for va, bva in vaugr.items:
            fw.op(pool, lambda va=va: nc.gpsimd.memset(va[:, :, 128:129], 1.0), writes=[bva])
        for h in range(4):
            kth, bk = kthr.next()
            fw.dma(sp, kth, KT[h], reads=KTb[h], writes=[bk])
            va, bva = vaugr.next()
            vsv = VS.rearrange("(i p) c -> p i c", p=128)
            for i0 in range(0, NT, 6):
                i1 = min(NT, i0 + 6)
                fw.dma(sp, va[:, i0:i1, 0:128], vsv[:, i0:i1, h * 128:(h + 1) * 128], reads=VSb[i0:i1], writes=[bva])
            qblocks = []
            if need_ctx:
                qblocks += [(t0, min(QB, C - t0), NCT) for t0 in range(0, C, QB)]
            qblocks += [(C + t0, min(QB, T - t0), NT) for t0 in range(0, T, QB)]
            for (t0, n, nkt) in qblocks:
                nq = n // 128
                qb, bq = qbr.next()
                fw.dma(sp, qb[:, 0:n], QT[h, :, t0:t0 + n], reads=QTb[h], writes=[bq])
                o0, bo0 = o0r.next()
                ytb, bytb = ytr.next()
                for c in range(2):
                    Sq = {}

                    def issue_S(kt):
                        pS, bS = psum(4, 8)
                        fw.op(pe, lambda: nc.tensor.matmul(pS[:, 0:n], kth[64 * c:64 * c + 64, kt * 128:(kt + 1) * 128], qb[64 * c:64 * c + 64, 0:n],
                                                           start=True, stop=True, tile_position=(64 * c, 0)),
                              reads=[bk, bq], writes=[bS])
                        Sq[kt] = (pS, bS)
                    issue_S(0)
                    if nkt > 1:
                        issue_S(1)
                    for kt in range(nkt):
                        pS, bS = Sq.pop(kt)
                        e, be = er.next()
                        fw.op(act, lambda: nc.scalar.activation(e[:, 0:n], pS[:, 0:n], AF.Exp, scale=0.125), reads=[bS], writes=[be])
                        if kt + 2 < nkt:
                            issue_S(kt + 2)
                        for qt in range(nq):
                            pa, bpa = PS[qt]
                            fw.op(pe, lambda: nc.tensor.matmul(pa[:, 0:129], e[:, qt * 128:(qt + 1) * 128], va[:, kt, :],
                                                               start=(kt == 0), stop=(kt == nkt - 1)),
                                  reads=[be, bva], writes=[bpa], inc=(kt == nkt - 1))
                    for qt in range(nq):
                        pa, bpa = PS[qt]
                        sm, bsm = smr.next()
                        fw.op(dve, lambda: nc.vector.reciprocal(sm[:, 0:1], pa[:, 128:129]), reads=[bpa], writes=[bsm])
                        if c == 0:
                            fw.op(dve, lambda: nc.vector.tensor_scalar(o0[:, qt, :], pa[:, 0:128], sm[:, 0:1], None, ALU.mult), reads=[bpa, bsm], writes=[bo0])
                            continue
                        fw.op(dve, lambda: nc.vector.tensor_tensor(sm[:, 2:3], sm[:, 0:1], lamT[:, 5:6], ALU.mult), reads=[bsm, b_lam], writes=[bsm])
                        fw.op(dve, lambda: nc.vector.scalar_tensor_tensor(o0[:, qt, :], pa[:, 0:128], sm[:, 2:3], o0[:, qt, :], ALU.mult, ALU.add),
                              reads=[bpa, bsm, bo0], writes=[bo0])
                        r, br = rstd_of(o0[:, qt, :], bo0, 128, 128)
                        y, by = yr.next()
                        fw.op(dve, lambda: nc.vector.scalar_tensor_tensor(y, o0[:, qt, :], r, ANW, ALU.mult, ALU.mult), reads=[bo0, br, b_nw], writes=[by])
                        pT, bpT = psum(4, 8)
                        pTv = pT.bitcast(BF16)
                        fw.op(pe, lambda: nc.tensor.transpose(pTv[:, 0:128], y, identb), reads=[by, b_cb], writes=[bpT])
                        fw.op(act, lambda: nc.scalar.copy(ytb[:, qt * 128:(qt + 1) * 128], pTv[:, 0:128]), reads=[bpT], writes=[bytb])
                fw.dma(pool, YT[h, :, t0:t0 + n], ytb[:, 0:n], reads=[bytb], writes=[YTb[h][t0 // 128 + j] for j in range(nq)])

    phase_region()
    LBt = sbt("LBt", [128, 2, 2, 512], F32)
    lbtmp = sbt("lbtmp", [128, 2, 512], F32)
    glf = Ring(sbt, "glf", [128, 512], F32, 22)
    glb = Ring(sbt, "glb", [128, 512], BF16, 12)
    gtt = Ring(sbt, "gtt", [128, 4, 128], BF16, 8)
    sbr = Ring(sbt, "sbr", [128, 4, 128], BF16, 4)
    Sst = sbt("Sst", [128, 4, 128], F32); b_S = Buf()
    e16r = Ring(sbt, "e16", [128, 4, 4], F32, 4)
    ss4r = Ring(sbt, "ss4", [128, 12], F32, 3)

    def gla(l, need_ctx):
        lb_consts(l)
        for d_ in (1, 0):
            Ld = Lb_f if d_ else Lf_f
            nm = nmask[:, 1, :] if d_ else nmask[:, 0, :]
            zslot = 3 if d_ else 2
            order = list(range(NCT)) + list(range(NCT, NT))
            if d_:
                order = list(range(NCT - 1, -1, -1)) + list(range(NT - 1, NCT - 1, -1))
            fw.op(dve, lambda: nc.vector.memset(Sst.rearrange("p a b -> p (a b)"), 0.0), reads=[b_S], writes=[b_S])
            st = {}
            st["sb"] = sbr.next()
            fw.op(dve, lambda: nc.vector.memset(st["sb"][0].rearrange("p a b -> p (a b)"), 0.0), writes=[st["sb"][1]])

            def prep(i):
                P = {"i": i, "need_o": need_ctx or i >= NCT}
                rows = slice(i * 128, (i + 1) * 128)
                P["rows"] = rows
                rq, brq = glf.next(); fw.dma(sp, rq, REC[rows, 0, :], reads=[RECb[i][0]], writes=[brq])
                ri, bri = glf.next(); fw.dma(sp, ri, REC[rows, 1, :], reads=[RECb[i][1]], writes=[bri])
                z, bz = glf.next(); fw.dma(sp, z, REC[rows, zslot, :], reads=[RECb[i][zslot]], writes=[bz])
                if P["need_o"] and not d_:
                    P["obt"] = glf.next(); fw.dma(sp, P["obt"][0], OB[rows, :], reads=[OBb[i]], writes=[P["obt"][1]])
                    P["og"] = glf.next(); fw.dma(sp, P["og"][0], REC[rows, 4, :], reads=[RECb[i][4]], writes=[P["og"][1]])
                fw.op(act, lambda: nc.scalar.activation(z, z, AF.Sigmoid), reads=[bz], writes=[bz])
                fw.op(dve, lambda: nc.vector.tensor_tensor(z, z, LBt[:, d_, 1, :], ALU.mult), reads=[bz, b_lb], writes=[bz])
                fw.op(dve, lambda: nc.vector.tensor_tensor(z, z, LBt[:, d_, 0, :], ALU.add), reads=[bz, b_lb], writes=[bz])
                lf, blf = glf.next()
                fw.op(act, lambda: nc.scalar.activation(lf, z, AF.Ln), reads=[bz], writes=[blf])
                pc, bpc = psum(4, 8)
                fw.op(pe, lambda: nc.tensor.matmul(pc, Ld, lf, start=True, stop=True), reads=[b_cm, blf], writes=[bpc])
                pE, bpE = psum(4, 8)
                for h in range(4):
                    fw.op(pe, lambda h=h: nc.tensor.matmul(pE[:, h * 4:(h + 1) * 4], lf[:, h * 128:(h + 1) * 128], indf, start=True, stop=True),
                          reads=[blf, b_cm], writes=[bpE], inc=(h == 3))
                E, bE = e16r.next()
                fw.op(act, lambda: nc.scalar.activation(E.rearrange("p a b -> p (a b)"), pE[:, 0:16], AF.Exp), reads=[bpE], writes=[bE])
                P["E"] = (E, bE)
                enc, benc = glf.next()
                fw.op(act, lambda: nc.scalar.activation(enc, pc, AF.Exp, scale=-1.0), reads=[bpc], writes=[benc])
                ktl, bktl = glb.next()
                fw.op(dve, lambda: nc.vector.scalar_tensor_tensor(ktl, z, 1.0, enc, ALU.subtract, ALU.mult), reads=[bz, benc], writes=[bktl])
                P["ktl"] = (ktl, bktl)
                vb, bvb = glb.next()
                fw.op(pool, lambda: nc.gpsimd.tensor_copy(vb, ri), reads=[bri], writes=[bvb])
                P["vb"] = (vb, bvb)
                if P["need_o"]:
                    ec, bec = glf.next()
                    fw.op(act, lambda: nc.scalar.activation(ec, pc, AF.Exp), reads=[bpc], writes=[bec])
                    fw.op(act, lambda: nc.scalar.activation(rq, rq, AF.Silu), reads=[brq], writes=[brq])
                    qtl, bqtl = glb.next()
                    fw.op(dve, lambda: nc.vector.tensor_tensor(qtl, rq, ec, ALU.mult), reads=[brq, bec], writes=[bqtl])
                    qtt, bqtt = gtt.next(); transpose_to(qtt, bqtt, qtl, bqtl, 4, 4, 8)
                    ktt, bktt = gtt.next(); transpose_to(ktt, bktt, ktl, bktl, 4, 4, 8)
                    P["qtt"] = (qtt, bqtt)
                    psc, bpsc = psum(4, 8)
                    for h in range(4):
                        fw.op(pe, lambda h=h: nc.tensor.matmul(psc[:, h * 128:(h + 1) * 128], ktt[:, h, :], qtt[:, h, :], start=True, stop=True),
                              reads=[bktt, bqtt], writes=[bpsc], inc=(h == 3))
                    sc, bsc = glb.next()
                    fw.op(dve, lambda: nc.vector.tensor_tensor(sc.rearrange("p (h t) -> p h t", h=4), psc.rearrange("p (h t) -> p h t", h=4),
                                                               nm.unsqueeze(1).to_broadcast([128, 4, 128]), ALU.mult),
                          reads=[bpsc, b_nm], writes=[bsc])
                    P["sc"] = (sc, bsc)
                    if not d_:
                        og, bog = P["og"]
                        fw.op(act, lambda: nc.scalar.activation(og, og, AF.Silu), reads=[bog], writes=[bog])
                return P

            def chain(P):
                i = P["i"]; rows = P["rows"]; need_o = P["need_o"]
                ktl, bktl = P["ktl"]; vb, bvb = P["vb"]; E, bE = P["E"]
                if need_o:
                    sc, bsc = P["sc"]; qtt, bqtt = P["qtt"]
                    for h in range(4):
                        fw.op(pe, lambda h=h: nc.tensor.matmul(PS[h][0][:, 0:128], sc[:, h * 128:(h + 1) * 128], vb[:, h * 128:(h + 1) * 128],
                                                               start=True, stop=False),
                              reads=[bsc, bvb], writes=[PS[h][1]], inc=False)
                corder = (3, 2, 1, 0) if d_ else (0, 1, 2, 3)
                for ci, c in enumerate(corder):
                    cs = slice(32 * c, 32 * c + 32)
                    sb_cur, bsb_cur = st["sb"]
                    if need_o:
                        for h in range(4):
                            fw.op(pe, lambda h=h: nc.tensor.matmul(PS[h][0][cs, 0:128], qtt[:, h, cs], sb_cur[:, h, :],
                                                                   start=False, stop=(ci == 3), tile_position=(0, 32 * c)),
                                  reads=[bqtt, bsb_cur], writes=[PS[h][1]], inc=(ci == 3))
                    pP, bpP = psum(4, 8)
                    for h in range(4):
                        fw.op(pe, lambda h=h: nc.tensor.matmul(pP[:, h * 128:(h + 1) * 128], ktl[cs, h * 128:(h + 1) * 128], vb[cs, h * 128:(h + 1) * 128],
                                                               start=True, stop=True, tile_position=(32 * c, 0)),
                              reads=[bktl, bvb], writes=[bpP], inc=(h == 3))
                    S2 = Sst.rearrange("p a b -> p (a b)")
                    fw.op(dve, lambda: nc.vector.tensor_tensor(S2, S2, pP, ALU.subtract), reads=[b_S, bpP], writes=[b_S])
                    fw.op(dve, lambda: nc.vector.tensor_tensor(Sst, Sst, E[:, :, c:c + 1].to_broadcast([128, 4, 128]), ALU.mult),
                          reads=[b_S, bE], writes=[b_S])
                    st["sb"] = sbr.next()
                    fw.op(act, lambda: nc.scalar.copy(st["sb"][0], Sst), reads=[b_S], writes=[st["sb"][1]])
                if not need_o:
                    return
                if d_:
                    of, bof = glf.next()
                    for h in range(4):
                        fw.op(act, lambda h=h: nc.scalar.copy(of[:, h * 128:(h + 1) * 128], PS[h][0][:, 0:128]), reads=[PS[h][1]], writes=[bof])
                    fw.dma(pool, OB[rows, :], of, reads=[bof], writes=[OBb[i]])
                else:
                    obt, bobt = P["obt"]; og, bog = P["og"]
                    for h in range(4):
                        fw.op(dve, lambda h=h: nc.vector.tensor_tensor(obt[:, h * 128:(h + 1) * 128], obt[:, h * 128:(h + 1) * 128], PS[h][0][:, 0:128], ALU.add),
                              reads=[bobt, PS[h][1]], writes=[bobt])
                    s4, bs4 = ss4r.next()
                    jk, bjk = junk.next()
                    fw.op(dve, lambda: nc.vector.memset(s4, 0.0), writes=[bs4])
                    for h in range(4):
                        fw.op(act, lambda h=h: nc.scalar.activation(jk[:, h * 128:(h + 1) * 128], obt[:, h * 128:(h + 1) * 128], AF.Square,
                                                                    accum_out=s4[:, h:h + 1]), reads=[bobt, bs4], writes=[bjk, bs4])
                    fw.op(act, lambda: nc.scalar.activation(s4[:, 4:8], s4[:, 0:4], AF.Sqrt, bias=epsT[:, 0:1], scale=1.0 / 128),
                          reads=[bs4, b_eps], writes=[bs4])
                    fw.op(dve, lambda: nc.vector.reciprocal(s4[:, 8:12], s4[:, 4:8]), reads=[bs4], writes=[bs4])
                    o3 = obt.rearrange("p (h v) -> p h v", h=4)
                    fw.op(dve, lambda: nc.vector.tensor_tensor(o3, o3, s4[:, 8:12].unsqueeze(2).to_broadcast([128, 4, 128]), ALU.mult),
                          reads=[bobt, bs4], writes=[bobt])
                    fw.op(dve, lambda: nc.vector.tensor_tensor(o3, o3, RNW.unsqueeze(1).to_broadcast([128, 4, 128]), ALU.mult),
                          reads=[bobt, b_nw], writes=[bobt])
                    yb, byb = glb.next()
                    fw.op(pool, lambda: nc.gpsimd.tensor_tensor(yb, obt, og, ALU.mult), reads=[bobt, bog], writes=[byb])
                    ytt, bytt = gtt.next(); transpose_to(ytt, bytt, yb, byb, 4, 4, 8)
                    fw.dma(pool, YT[4:8, :, rows].rearrange("h p t -> p h t"), ytt, reads=[bytt], writes=[YTb[4 + h][i] for h in range(4)])

            nxt = prep(order[0])
            for k_, i in enumerate(order):
                cur = nxt
                nxt = prep(order[k_ + 1]) if k_ + 1 < len(order) else None
                chain(cur)

    phase_region()
    M2 = {k: (sbt(f"m2_{k}", [128, D], F32), Buf(k)) for k in ["g2l", "g2c"]}
    WUA = sbt("WUA", [128, 4, D], BF16); WUR = sbt("WUR", [128, 4, D], BF16); WOUT = sbt("WOUT", [128, KC, D], BF16); b_wm = Buf()
    ytbr = Ring(sbt, "ytblk", [128, 8, 512], BF16, 2)
    gtbr = Ring(sbt, "gtblk", [128, 16, 512], BF16, 2)
    mTr = Ring(sbt, "mT", [128, KC, 512], BF16, 2)

    def merge(l, b, need_ctx):
        load_mod(*M2["g2l"], l, b, 2)
        load_mod(*M2["g2c"], l, NB, 2)
        fw.dma(sp, WUA, wua_b[l].rearrange("(kc p) n -> p kc n", p=128), writes=[b_wm])
        fw.dma(sp, WUR, wur_b[l].rearrange("(kc p) n -> p kc n", p=128), writes=[b_wm])
        fw.dma(sp, WOUT, wout_b[l].rearrange("(kc p) n -> p kc n", p=128), writes=[b_wm])
        for bi, (t0, n) in enumerate(blocks_all):
            if bi == 0 and not need_ctx:
                continue
            tiles = list(range(t0 // 128, (t0 + n) // 128))
            ytb, bytb = ytbr.next()
            fw.dma(sp, ytb[:, :, 0:n], YT[:, :, t0:t0 + n].rearrange("h p t -> p h t"),
                   reads=[YTb[h][i] for h in range(8) for i in tiles], writes=[bytb])
            gtb, bgtb = gtbr.next()
            fw.dma(sp, gtb[:, :, 0:n], GT[:, :, t0:t0 + n].rearrange("h p t -> p h t"), reads=[GTb[g][bi] for g in range(16)], writes=[bgtb])
            mT, bmT = mTr.next()
            for m in range(8):
                pa, bpa = psum(); pb, bpb = psum()
                for kc in range(4):
                    fw.op(pe, lambda kc=kc: nc.tensor.matmul(pa[:, 0:n], WUA[:, kc, m * 128:(m + 1) * 128], ytb[:, kc, 0:n], start=(kc == 0), stop=(kc == 3)),
                          reads=[b_wm, bytb], writes=[bpa], inc=(kc == 3))
                for kc in range(4):
                    fw.op(pe, lambda kc=kc: nc.tensor.matmul(pb[:, 0:n], WUR[:, kc, m * 128:(m + 1) * 128], ytb[:, 4 + kc, 0:n], start=(kc == 0), stop=(kc == 3)),
                          reads=[b_wm, bytb], writes=[bpb], inc=(kc == 3))
                t1, bt1 = f32r.next(); t2, bt2 = f32r.next()
                fw.op(dve, lambda: nc.vector.tensor_tensor(t1[:, 0:n], pa[:, 0:n], gtb[:, m, 0:n], ALU.mult), reads=[bpa, bgtb], writes=[bt1])
                fw.op(dve, lambda: nc.vector.tensor_tensor(t2[:, 0:n], pb[:, 0:n], gtb[:, 8 + m, 0:n], ALU.mult), reads=[bpb, bgtb], writes=[bt2])
                fw.op(pool, lambda: nc.gpsimd.tensor_tensor(mT[:, m, 0:n], t1[:, 0:n], t2[:, 0:n], ALU.add), reads=[bt1, bt2], writes=[bmT])
            suf = "c" if bi == 0 else "l"
            g2, bg2 = M2["g2" + suf]
            for j, i in enumerate(tiles):
                xt, bx = xr.next()
                fw.dma(sp, xt, xsrc(l, b, i), reads=[XSb[i]], writes=[bx])
                xo, bxo = xor_.next()
                for hf in range(2):
                    py, bpy = psum()
                    for kc in range(KC):
                        fw.op(pe, lambda kc=kc: nc.tensor.matmul(py, mT[:, kc, j * 128:(j + 1) * 128], WOUT[:, kc, hf * 512:(hf + 1) * 512],
                                                                 start=(kc == 0), stop=(kc == KC - 1)),
                              reads=[bmT, b_wm], writes=[bpy], inc=(kc == KC - 1))
                    cs = slice(hf * 512, (hf + 1) * 512)
                    fw.op(dve, lambda: nc.vector.tensor_tensor(xo[:, cs], py, g2[:, cs], ALU.mult), reads=[bpy, bg2], writes=[bxo])
                    fw.op(pool, lambda: nc.gpsimd.tensor_tensor(xo[:, cs], xo[:, cs], xt[:, cs], ALU.add), reads=[bxo, bx], writes=[bxo])
                fw.dma(pool, XS[i * 128:(i + 1) * 128, :], xo, reads=[bxo], writes=[XSb[i]])

    NCH_D = (DFF + 127) // 128

    def ffn_set(nblk, dense):
        phase_region()
        F_ = {}
        F_["M"] = {k: (sbt(f"m5_{k}{int(dense)}", [128, D], F32), Buf(k)) for k in ["G2", "S2", "g5"]}
        F_["h2T"] = sbt(f"h2T{int(dense)}", [128, KC, nblk], BF16); F_["b_h2"] = [Buf() for _ in range(nblk // 128)]
        F_["Xblk"] = sbt(f"Xblk{int(dense)}", [128, nblk // 128, D], F32); F_["b_xb"] = [Buf() for _ in range(nblk // 128)]
        F_["aT"] = sbt(f"aT{int(dense)}", [128, 1, 4 if dense else 7, nblk], BF16); F_["b_aT"] = [Buf()]
        F_["w13r"] = Ring(sbt, f"w13{int(dense)}", [128, KC, 512], BF16, 4)
        F_["silr"] = Ring(sbt, f"sil{int(dense)}", [128, 512], F32, 3)
        if dense:
            F_["W2D"] = sbt("W2D", [128, NCH_D, D], BF16); F_["b_w2d"] = Buf()
        else:
            F_["w2r"] = Ring(sbt, "w2q", [128, 7, D], BF16, 2)
            F_["G8"] = sbt("G8", [128, nblk // 128, 8], F32); F_["b_g8"] = Buf()
            F_["rt_sb"] = sbt("rt_sb", [128, KC, NE], BF16); F_["b_rt"] = Buf()
            F_["lgr"] = Ring(sbt, "lg", [128, 32], F32, 3)
            F_["FNW"] = sbt("FNW", [128, D], F32)
        return F_

    FD = ffn_set(512, True) if n_dense else None
    MB = 1024 if T >= 1024 else T
    FM = ffn_set(MB, False) if n_moe else None
    Fc = {}

    def use_set(F_):
        Fc.clear(); Fc.update(F_)

    def load_ffn_mods(l, row):
        M = Fc["M"]
        load_gain_shift(*M["G2"], *M["S2"], l, row, 4, 3, nfw)
        load_mod(*M["g5"], l, row, 5)

    def load_block(l, b, t0, n):
        h2T, Xblk, b_xb, b_h2, M = Fc["h2T"], Fc["Xblk"], Fc["b_xb"], Fc["b_h2"], Fc["M"]
        for j in range(n // 128):
            i = t0 // 128 + j
            fw.dma(sp, Xblk[:, j, :], XS[i * 128:(i + 1) * 128, :], reads=[XSb[i]], writes=[b_xb[j]])
            hb, bhb = hbr.next()
            norm_mod(Xblk[:, j, :], b_xb[j], *M["G2"], *M["S2"], hb, bhb)
            transpose_to(h2T[:, :, j * 128:(j + 1) * 128], b_h2[j], hb, bhb, KC)

    def swiglu_group(w1v, w3v, c0, ncols, n, choff):
        h2T, b_h2, aT, b_aT, w13r, silr = Fc["h2T"], Fc["b_h2"], Fc["aT"], Fc["b_aT"], Fc["w13r"], Fc["silr"]
        s1, bs1 = w13r.next(); s3, bs3 = w13r.next()
        fw.dma(sp, s1[:, :, 0:ncols], w1v[:, :, c0:c0 + ncols], writes=[bs1])
        fw.dma(sp, s3[:, :, 0:ncols], w3v[:, :, c0:c0 + ncols], writes=[bs3])
        nch = (ncols + 127) // 128
        for cc in range(nch):
            w = min(128, ncols - cc * 128)
            for tb in range(0, n, 512):
                nn = min(512, n - tb)
                tl = [b_h2[j] for j in range(tb // 128, (tb + nn) // 128)]
                p1, bp1 = psum(); p3, bp3 = psum()
                for kc in range(KC):
                    fw.op(pe, lambda kc=kc: nc.tensor.matmul(p1[0:w, 0:nn], s1[:, kc, cc * 128:cc * 128 + w], h2T[:, kc, tb:tb + nn],
                                                             start=(kc == 0), stop=(kc == KC - 1)),
                          reads=[bs1] + tl, writes=[bp1], inc=(kc == KC - 1))
                for kc in range(KC):
                    fw.op(pe, lambda kc=kc: nc.tensor.matmul(p3[0:w, 0:nn], s3[:, kc, cc * 128:cc * 128 + w], h2T[:, kc, tb:tb + nn],
                                                             start=(kc == 0), stop=(kc == KC - 1)),
                          reads=[bs3] + tl, writes=[bp3], inc=(kc == KC - 1))
                sl_, bsl_ = silr.next()
                fw.op(act, lambda: nc.scalar.activation(sl_[0:w, 0:nn], p1[0:w, 0:nn], AF.Silu), reads=[bp1], writes=[bsl_])
                fw.op(dve, lambda: nc.vector.tensor_tensor(aT[0:w, 0, choff + cc, tb:tb + nn], sl_[0:w, 0:nn], p3[0:w, 0:nn], ALU.mult),
                      reads=[bsl_, bp3], writes=[b_aT[0]])

    def down_proj(w2t, bw2, ch0, nch, ncols, n, scale_fn):
        aT, b_aT, Xblk, b_xb = Fc["aT"], Fc["b_aT"], Fc["Xblk"], Fc["b_xb"]
        for j in range(n // 128):
            for hf in range(2):
                py, bpy = psum()
                for cc in range(nch):
                    w = min(128, ncols - cc * 128)
                    fw.op(pe, lambda: nc.tensor.matmul(py, aT[0:w, 0, cc, j * 128:(j + 1) * 128], w2t[0:w, ch0 + cc, hf * 512:(hf + 1) * 512],
                                                       start=(cc == 0), stop=(cc == nch - 1)),
                          reads=[b_aT[0], bw2], writes=[bpy], inc=(cc == nch - 1))
                cs = slice(hf * 512, (hf + 1) * 512)
                tq, btq = f32r.next()
                scale_fn(tq, btq, py, bpy, j, cs)
                fw.op(pool, lambda: nc.gpsimd.tensor_tensor(Xblk[:, j, cs], Xblk[:, j, cs], tq, ALU.add), reads=[btq, b_xb[j]], writes=[b_xb[j]])

    def store_block(t0, n):
        Xblk, b_xb = Fc["Xblk"], Fc["b_xb"]
        for j in range(n // 128):
            i = t0 // 128 + j
            fw.dma(pool, XS[i * 128:(i + 1) * 128, :], Xblk[:, j, :], reads=[b_xb[j]], writes=[XSb[i]])

    def ffn_dense(l, b, need_ctx):
        use_set(FD)
        W2D, b_w2d = Fc["W2D"], Fc["b_w2d"]
        j_ = l // 2
        w1v = fw1_b[j_].rearrange("(kc p) f -> p kc f", p=128); w3v = fw3_b[j_].rearrange("(kc p) f -> p kc f", p=128)
        nfull = DFF // 128
        for c0_ in range(0, nfull, 8):
            c1_ = min(nfull, c0_ + 8)
            fw.dma(sp, W2D[:, c0_:c1_, :], fw2_b[j_, c0_ * 128:c1_ * 128, :].rearrange("(ch p) n -> p ch n", p=128), writes=[b_w2d])
        if DFF % 128:
            fw.dma(sp, W2D[0:DFF % 128, nfull, :], fw2_b[j_, nfull * 128:DFF, :], writes=[b_w2d])
        groups = [(c0, min(512, DFF - c0)) for c0 in range(0, DFF, 512)]
        cur_row = None
        for bi, (t0, n) in enumerate(blocks_all):
            if bi == 0 and not need_ctx:
                continue
            row = NB if bi == 0 else b
            if row != cur_row:
                load_ffn_mods(l, row); cur_row = row
            load_block(l, b, t0, n)
            g5, bg5 = Fc["M"]["g5"]

            def sc(tq, btq, py, bpy, j, cs):
                fw.op(dve, lambda: nc.vector.tensor_tensor(tq, py, g5[:, cs], ALU.mult), reads=[bpy, bg5], writes=[btq])
            for (c0, ncols) in groups:
                swiglu_group(w1v, w3v, c0, ncols, n, 0)
                down_proj(W2D, b_w2d, c0 // 128, (ncols + 127) // 128, ncols, n, sc)
            store_block(t0, n)

    def final_norm_store(b, t0, n):
        Xblk, b_xb, FNW = Fc["Xblk"], Fc["b_xb"], Fc["FNW"]
        for j in range(n // 128):
            r, br = rstd_of(Xblk[:, j, :], b_xb[j], D, D)
            xo, bxo = xor_.next()
            fw.op(dve, lambda: nc.vector.scalar_tensor_tensor(xo, Xblk[:, j, :], r, FNW, ALU.mult, ALU.mult), reads=[b_xb[j], br, b_fnw], writes=[bxo])
            lt = t0 - C + j * 128
            out_toks.append(fw.dma(pool, out_d[b, lt:lt + 128, :], xo, reads=[bxo], writes=[Buf()]))

    def ffn_moe(l, b, need_ctx, last):
        use_set(FM)
        h2T, b_h2, G8, b_g8, rt_sb, b_rt, lgr, w2r = Fc["h2T"], Fc["b_h2"], Fc["G8"], Fc["b_g8"], Fc["rt_sb"], Fc["b_rt"], Fc["lgr"], Fc["w2r"]
        j_ = l // 2
        fw.dma(sp, rt_sb, rtw_b[j_].rearrange("(kc p) e -> p kc e", p=128), writes=[b_rt])
        fw.dma(sp, Fc["FNW"], bc(fnw[0:1, :]), writes=[b_fnw])
        mblocks = []
        if need_ctx:
            mblocks.append((0, C, NB))
        mblocks += [(C + t, min(MB, T - t), b) for t in range(0, T, MB)]
        cur_row = None
        for (t0, n, row) in mblocks:
            if row != cur_row:
                load_ffn_mods(l, row); cur_row = row
            load_block(l, b, t0, n)
            g5, bg5 = Fc["M"]["g5"]
            ntl = n // 128
            for j in range(ntl):
                pl, bpl = psum()
                for kc in range(KC):
                    fw.op(pe, lambda kc=kc: nc.tensor.matmul(pl[:, 0:NE], h2T[:, kc, j * 128:(j + 1) * 128], rt_sb[:, kc, :],
                                                             start=(kc == 0), stop=(kc == KC - 1)),
                          reads=[b_h2[j], b_rt], writes=[bpl], inc=(kc == KC - 1))
                lg, blg = lgr.next()
                fw.op(act, lambda: nc.scalar.copy(lg[:, 0:8], pl[:, 0:8]), reads=[bpl], writes=[blg])
                fw.op(dve, lambda: nc.vector.max(lg[:, 8:16], lg[:, 0:8]), reads=[blg], writes=[blg])
                fw.op(dve, lambda: nc.vector.tensor_scalar(lg[:, 16:24], lg[:, 0:8], lg[:, 9:10], None, ALU.is_ge), reads=[blg], writes=[blg])
                fw.op(dve, lambda: nc.vector.tensor_scalar(lg[:, 24:25], lg[:, 8:9], -1.0, None, ALU.mult), reads=[blg], writes=[blg])
                fw.op(act, lambda: nc.scalar.activation(lg[:, 0:8], lg[:, 0:8], AF.Exp, bias=lg[:, 24:25], scale=1.0), reads=[blg], writes=[blg])
                fw.op(dve, lambda: nc.vector.tensor_tensor(lg[:, 0:8], lg[:, 0:8], lg[:, 16:24], ALU.mult), reads=[blg], writes=[blg])
                fw.op(dve, lambda: nc.vector.reduce_sum(lg[:, 25:26], lg[:, 0:8], AX.X), reads=[blg], writes=[blg])
                fw.op(dve, lambda: nc.vector.reciprocal(lg[:, 26:27], lg[:, 25:26]), reads=[blg], writes=[blg])
                fw.op(dve, lambda: nc.vector.tensor_scalar(G8[:, j, :], lg[:, 0:8], lg[:, 26:27], None, ALU.mult), reads=[blg, b_g8], writes=[b_g8])
            for e in range(NE):
                w1v = mw1_b[j_, e].rearrange("(kc p) f -> p kc f", p=128); w3v = mw3_b[j_, e].rearrange("(kc p) f -> p kc f", p=128)
                QW = 896

                def sc(tq, btq, py, bpy, j, cs, e=e):
                    fw.op(dve, lambda: nc.vector.scalar_tensor_tensor(tq, py, G8[:, j, e:e + 1], g5[:, cs], ALU.mult, ALU.mult),
                          reads=[bpy, b_g8, bg5], writes=[btq])
                for c0 in range(0, DFE, QW):
                    ncols = min(QW, DFE - c0)
                    for g0 in range(0, ncols, 512):
                        swiglu_group(w1v, w3v, c0 + g0, min(512, ncols - g0), n, g0 // 128)
                    nch = (ncols + 127) // 128
                    w2q, bw2q = w2r.next()
                    nfull = ncols // 128
                    if nfull:
                        fw.dma(sp, w2q[:, 0:nfull, :], mw2_b[j_, e, c0:c0 + nfull * 128, :].rearrange("(ch p) n -> p ch n", p=128), writes=[bw2q])
                    if ncols % 128:
                        fw.dma(sp, w2q[0:ncols % 128, nfull, :], mw2_b[j_, e, c0 + nfull * 128:c0 + ncols, :], writes=[bw2q])
                    down_proj(w2q, bw2q, 0, nch, ncols, n, sc)
            if last:
                if row != NB:
                    final_norm_store(b, t0, n)
            else:
                store_block(t0, n)

    def barrier():
        fw.barrier()

    stages = []
    done = False
    for b in range(NB):
        for l in range(L):
            last = (l == L - 1)
            need_ctx = not last
            layer_consts(l)
            for name, f in (("p1", lambda: (phase1(l, b), phase1b(l, b))), ("att", lambda: attention(l, need_ctx)),
                            ("gla", lambda: gla(l, need_ctx)), ("merge", lambda: merge(l, b, need_ctx)),
                            ("ffn", lambda: (ffn_dense(l, b, need_ctx) if l % 2 == 0 else ffn_moe(l, b, need_ctx, last)))):
                barrier()
                if name == "ffn" and l % 2 == 1:
                    need_moe_weights()
                f()
                if b == 0 and l == 0:
                    inject_conv(12)
                if stop_after == (b, l, name):
                    done = True
                    break
            if done:
                break
        if done:
            break
    barrier()
    if dbg:
        for i in range(NT):
            out_toks.append(fw.dma(sp, dbg_d[i * 128:(i + 1) * 128, :], XS[i * 128:(i + 1) * 128, :], reads=[XSb[i]], writes=[Buf()]))
    fw.finish(out_toks, sp)
    fw.finish(out_toks, pool)
    fw.close()
    info = {"n_inst": fw.n_inst, "sbuf_hi": reg["hi"], "sbuf_top": reg["top"]}
    return nc, info


_PROG_CACHE = {}


def _inputs_for_core(inp, b0, NB, consts):
    m = {
        "x": np.ascontiguousarray(inp["x"][b0:b0 + NB]),
        "ctx": np.ascontiguousarray(inp["ctx"][b0:b0 + NB]),
        "cc": np.ascontiguousarray(np.concatenate([inp["c"][b0:b0 + NB], inp["c_ctx"][None, :]], axis=0)),
        "final_norm_w": np.ascontiguousarray(inp["final_norm_w"][None, :]),
    }
    for k in ("w_ada", "b_ada", "norm_mix_w", "norm_ffn_w", "w_in", "lambda_q1", "lambda_k1", "lambda_q2", "lambda_k2",
              "att_norm_w", "rec_norm_w", "lb_fwd", "lb_bwd", "w_up_att", "w_up_rec", "w_out", "ffn_w1", "ffn_w3", "ffn_w2",
              "router_w", "moe_w1", "moe_w3", "moe_w2"):
        m[k] = inp[k]
    m.update(consts)
    return m


def kernel(**inputs):
    inp = {k: np.asarray(v) for k, v in inputs.items()}
    B, T, _ = inp["x"].shape
    C = inp["ctx"].shape[1]
    DFF = inp["ffn_w1"].shape[2]
    DFE = inp["moe_w1"].shape[3]
    ncores = 8
    NB = B // ncores
    key = (NB, T, C, DFF, DFE)
    if key not in _PROG_CACHE:
        _PROG_CACHE[key] = build_program(NB, T, C, DFF, DFE)[0]
    nc = _PROG_CACHE[key]
    consts = host_consts(T)
    in_maps = [_inputs_for_core(inp, c * NB, NB, consts) for c in range(ncores)]
    res = run_bass_kernel_spmd(nc, in_maps, core_ids=list(range(ncores)))
    return np.concatenate([np.asarray(r["out"]) for r in res.results], axis=0).astype(np.float32)
```
